# Optimizing a Trainium2 kernel written in Bass

```python
import math
import jax, jax.numpy as jnp
from jax import lax
import numpy as np

D_MODEL = 2048
BATCH = 4
SEQ = 2048
DEPTH = 1

HEAD_DIM = 64
ATT_WIDTH = D_MODEL // 2
ATT_HEADS = ATT_WIDTH // HEAD_DIM
ATT_KV_HEADS = ATT_HEADS // 4
KV_WIDTH = ATT_KV_HEADS * HEAD_DIM
WINDOW = 128
ATT_BLOCK = 128
ROPE_DIM = HEAD_DIM // 4
ROPE_THETA = 500000.0
SG_WIDTH = D_MODEL // 2
SG_GROUP_DIM = 128
SG_GROUPS = SG_WIDTH // SG_GROUP_DIM
SG_CHUNK = 128
N_BRANCHES = 2
OFF_Q = 0
OFF_K = OFF_Q + ATT_WIDTH
OFF_V = OFF_K + KV_WIDTH
OFF_U = OFF_V + KV_WIDTH
OFF_S = OFF_U + SG_WIDTH
OFF_G = OFF_S + SG_WIDTH
IN_COLS = OFF_G + N_BRANCHES * D_MODEL
N_GROUPS = 8
EXPERTS_PER_GROUP = 8
N_EXPERTS = N_GROUPS * EXPERTS_PER_GROUP
TOP_K = 2
EXPERT_FF = D_MODEL // 4
MOE_BLOCK = 128
EPS = 1e-6
NEG_INF = -1e30

kernel_name = "hybrid_gated_window_gqa_spatial_gating_hmoe"


def rms_norm(x, g):
    xf = x.astype(jnp.float32)
    r = lax.rsqrt(jnp.mean(xf * xf, axis=-1, keepdims=True) + EPS)
    return (xf * r).astype(x.dtype) * g


def layer_norm(x, g, b):
    xf = x.astype(jnp.float32)
    mu = jnp.mean(xf, axis=-1, keepdims=True)
    xc = xf - mu
    var = jnp.mean(xc * xc, axis=-1, keepdims=True)
    return (xc * lax.rsqrt(var + EPS)).astype(x.dtype) * g + b


def rope_tables(positions, dtype):
    inv = ROPE_THETA ** (-jnp.arange(0, ROPE_DIM, 2, dtype=jnp.float32) / ROPE_DIM)
    ang = positions.astype(jnp.float32)[..., None] * inv
    return jnp.cos(ang)[:, :, None, :].astype(dtype), jnp.sin(ang)[:, :, None, :].astype(dtype)


def partial_rope(t, cos, sin):
    half = ROPE_DIM // 2
    r1, r2, rest = t[..., :half], t[..., half:ROPE_DIM], t[..., ROPE_DIM:]
    return jnp.concatenate([r1 * cos - r2 * sin, r2 * cos + r1 * sin, rest], axis=-1)


def window_attention(q, k, v, sink):
    B, S = q.shape[0], q.shape[1]
    nb = S // ATT_BLOCK
    G = ATT_HEADS // ATT_KV_HEADS
    qb = q.reshape(B, nb, ATT_BLOCK, ATT_KV_HEADS, G, HEAD_DIM)

    def neighbours(t):
        tp = jnp.pad(t, ((0, 0), (ATT_BLOCK, ATT_BLOCK), (0, 0), (0, 0)))
        tp = tp.reshape(B, nb + 2, ATT_BLOCK, ATT_KV_HEADS, HEAD_DIM)
        return jnp.concatenate([tp[:, :-2], tp[:, 1:-1], tp[:, 2:]], axis=2)

    kb, vb = neighbours(k), neighbours(v)
    scores = jnp.einsum('bnqhgd,bnkhd->bnhgqk', qb, kb).astype(jnp.float32) * (HEAD_DIM ** -0.5)
    qi = jnp.arange(ATT_BLOCK)[:, None]
    kj = jnp.arange(3 * ATT_BLOCK)[None, :]
    band = jnp.abs(kj - ATT_BLOCK - qi) <= WINDOW
    key_pos = jnp.arange(nb)[:, None] * ATT_BLOCK + kj - ATT_BLOCK
    in_seq = (key_pos >= 0) & (key_pos < S)
    mask = band[None, :, :] & in_seq[:, None, :]
    scores = jnp.where(mask[None, :, None, None], scores, NEG_INF)
    sink_col = jnp.broadcast_to(
        sink.astype(jnp.float32).reshape(1, 1, ATT_KV_HEADS, G, 1, 1),
        scores.shape[:-1] + (1,))
    probs = jax.nn.softmax(jnp.concatenate([scores, sink_col], axis=-1), axis=-1)[..., :-1]
    out = jnp.einsum('bnhgqk,bnkhd->bnqhgd', probs.astype(v.dtype), vb)
    return out.reshape(B, S, ATT_WIDTH)


def spatial_gating(u, s, ln_g, ln_b, w_s, b_s):
    B, S = u.shape[0], u.shape[1]
    sg = layer_norm(s.reshape(B, S, SG_GROUPS, SG_GROUP_DIM), ln_g, ln_b)
    sc = sg.reshape(B, S // SG_CHUNK, SG_CHUNK, SG_GROUPS, SG_GROUP_DIM)
    mixed = jnp.einsum('gts,bnsgc->bntgc', w_s, sc) + b_s.T[None, None, :, :, None]
    return u * mixed.reshape(B, S, SG_WIDTH)


def hierarchical_moe(x, w_group, b_group, w_erouter, b_erouter, w_gate, w_up, w_down):
    B, S, D = x.shape
    T = B * S
    xt = x.reshape(T, D)
    g_logits = (xt @ w_group).astype(jnp.float32) + b_group
    g_prob = jax.nn.softmax(g_logits, axis=-1)
    grp = jnp.argmax(g_logits, axis=-1)
    g_w = jnp.take_along_axis(g_prob, grp[:, None], axis=1)[:, 0]
    e_logits = ((xt @ w_erouter).astype(jnp.float32) + b_erouter).reshape(T, N_GROUPS, EXPERTS_PER_GROUP)
    e_logits = jnp.take_along_axis(e_logits, grp[:, None, None], axis=1)[:, 0]
    top_v, top_i = lax.top_k(e_logits, TOP_K)
    top_w = jax.nn.softmax(top_v, axis=-1) * g_w[:, None]
    expert = grp[:, None] * EXPERTS_PER_GROUP + top_i
    TK = T * TOP_K
    flat_e = expert.reshape(TK).astype(jnp.int32)
    flat_t = jnp.repeat(jnp.arange(T, dtype=jnp.int32), TOP_K)
    flat_w = top_w.reshape(TK)
    order = jnp.argsort(flat_e)
    se, st, sw = flat_e[order], flat_t[order], flat_w[order]
    counts = jnp.bincount(flat_e, length=N_EXPERTS)
    starts = jnp.cumsum(counts) - counts
    padded = (counts + MOE_BLOCK - 1) // MOE_BLOCK * MOE_BLOCK
    pad_ends = jnp.cumsum(padded)
    pad_starts = pad_ends - padded
    dest = pad_starts[se] + jnp.arange(TK) - starts[se]
    n_rows = TK + N_EXPERTS * MOE_BLOCK
    n_blocks = n_rows // MOE_BLOCK
    row_tok = jnp.full((n_rows,), T, jnp.int32).at[dest].set(st)
    row_w = jnp.zeros((n_rows,), jnp.float32).at[dest].set(sw)
    block_e = jnp.minimum(
        jnp.searchsorted(pad_ends, jnp.arange(n_blocks) * MOE_BLOCK, side='right'),
        N_EXPERTS - 1)
    xpad = jnp.concatenate([xt, jnp.zeros((1, D), xt.dtype)], axis=0)
    xrows = xpad[row_tok].reshape(n_blocks, MOE_BLOCK, D)

    def expert_block(args):
        xb, e = args
        hdn = jax.nn.silu(xb @ w_gate[e]) * (xb @ w_up[e])
        return hdn @ w_down[e]

    yrows = lax.map(expert_block, (xrows, block_e)).reshape(n_rows, D)
    yrows = yrows * row_w[:, None].astype(yrows.dtype)
    y = jax.ops.segment_sum(yrows, row_tok, num_segments=T + 1)[:T]
    return y.reshape(B, S, D)


def setup_inputs(seed: int = 0) -> dict:
    key = jax.random.key(seed)
    ks = jax.random.split(key, 24)
    f32 = jnp.float32

    def nrm(k, shape, scale):
        return jax.random.normal(k, shape, f32) * scale

    L = DEPTH
    x = jax.random.normal(ks[0], (BATCH, SEQ, D_MODEL), f32)
    offsets = jax.random.randint(ks[1], (BATCH, 1), 0, 4096, dtype=jnp.int32)
    positions = offsets + jnp.arange(SEQ, dtype=jnp.int32)[None, :]
    return {
        "x": x,
        "positions": positions,
        "norm1_g": 1.0 + nrm(ks[2], (L, D_MODEL), 0.02),
        "w_in": nrm(ks[3], (L, D_MODEL, IN_COLS), D_MODEL ** -0.5),
        "q_norm_g": 1.0 + nrm(ks[4], (L, HEAD_DIM), 0.02),
        "k_norm_g": 1.0 + nrm(ks[5], (L, HEAD_DIM), 0.02),
        "sink_logits": nrm(ks[6], (L, ATT_HEADS), 0.5),
        "sg_ln_g": 1.0 + nrm(ks[7], (L, SG_GROUPS, SG_GROUP_DIM), 0.02),
        "sg_ln_b": nrm(ks[8], (L, SG_GROUPS, SG_GROUP_DIM), 0.02),
        "sg_w": nrm(ks[9], (L, SG_GROUPS, SG_CHUNK, SG_CHUNK), SG_CHUNK ** -0.5),
        "sg_b": 1.0 + nrm(ks[10], (L, SG_GROUPS, SG_CHUNK), 0.02),
        "w_branch_att": nrm(ks[11], (L, ATT_WIDTH, D_MODEL), ATT_WIDTH ** -0.5),
        "w_branch_sg": nrm(ks[12], (L, SG_WIDTH, D_MODEL), SG_WIDTH ** -0.5),
        "w_out": nrm(ks[13], (L, D_MODEL, D_MODEL), D_MODEL ** -0.5),
        "norm2_g": 1.0 + nrm(ks[14], (L, D_MODEL), 0.02),
        "w_group_router": nrm(ks[15], (L, D_MODEL, N_GROUPS), D_MODEL ** -0.5),
        "b_group_router": nrm(ks[16], (L, N_GROUPS), 0.01),
        "w_expert_router": nrm(ks[17], (L, D_MODEL, N_EXPERTS), D_MODEL ** -0.5),
        "b_expert_router": nrm(ks[18], (L, N_EXPERTS), 0.01),
        "w_gate": nrm(ks[19], (L, N_EXPERTS, D_MODEL, EXPERT_FF), D_MODEL ** -0.5),
        "w_up": nrm(ks[20], (L, N_EXPERTS, D_MODEL, EXPERT_FF), D_MODEL ** -0.5),
        "w_down": nrm(ks[21], (L, N_EXPERTS, EXPERT_FF, D_MODEL), EXPERT_FF ** -0.5),
    }


def reference(x, positions, norm1_g, w_in, q_norm_g, k_norm_g, sink_logits, sg_ln_g, sg_ln_b,
              sg_w, sg_b, w_branch_att, w_branch_sg, w_out, norm2_g, w_group_router,
              b_group_router, w_expert_router, b_expert_router, w_gate, w_up, w_down):
    B, S, D = x.shape
    cos, sin = rope_tables(positions, x.dtype)
    h = x
    for l in range(DEPTH):
        xn = rms_norm(h, norm1_g[l])
        proj = xn @ w_in[l]
        q = proj[..., OFF_Q:OFF_K].reshape(B, S, ATT_HEADS, HEAD_DIM)
        k = proj[..., OFF_K:OFF_V].reshape(B, S, ATT_KV_HEADS, HEAD_DIM)
        v = proj[..., OFF_V:OFF_U].reshape(B, S, ATT_KV_HEADS, HEAD_DIM)
        u = jax.nn.gelu(proj[..., OFF_U:OFF_S])
        s = jax.nn.gelu(proj[..., OFF_S:OFF_G])
        gates = jax.nn.sigmoid(proj[..., OFF_G:]).reshape(B, S, N_BRANCHES, D)
        q = partial_rope(rms_norm(q, q_norm_g[l]), cos, sin)
        k = partial_rope(rms_norm(k, k_norm_g[l]), cos, sin)
        att = window_attention(q, k, v, sink_logits[l])
        sgo = spatial_gating(u, s, sg_ln_g[l], sg_ln_b[l], sg_w[l], sg_b[l])
        merged = (gates[:, :, 0, :] * (att @ w_branch_att[l])
                  + gates[:, :, 1, :] * (sgo @ w_branch_sg[l]))
        h = h + merged @ w_out[l]
        hn = rms_norm(h, norm2_g[l])
        h = h + hierarchical_moe(hn, w_group_router[l], b_group_router[l], w_expert_router[l],
                                 b_expert_router[l], w_gate[l], w_up[l], w_down[l])
    return h
```

```python
import math
import os
from contextlib import ExitStack

import numpy as np
import concourse.bass as bass
import concourse.mybir as mybir
from concourse.bass_utils import run_bass_kernel_spmd

F32 = mybir.dt.float32
BF16 = mybir.dt.bfloat16
I32 = mybir.dt.int32
AF = mybir.ActivationFunctionType
ALU = mybir.AluOpType
AX = mybir.AxisListType

NCORES = 8
D = 2048
TOK = 1024
NB = 8
NBH = 10
INC = 7680
OFF_K, OFF_V, OFF_U, OFF_S, OFF_G = 1024, 1280, 1536, 2560, 3584
NE = 64
CAP = 128
FF = 512
EPS = 1e-6
SB_BASE = 16512
SB_END = 229376
BIG = 1 << 30
TWO_PI = 2.0 * math.pi

DEBUG = os.environ.get("KDEBUG", "") != ""
STAGE = int(os.environ.get("KSTAGE", "9"))

ENGS = ("pe", "act", "dve", "pool", "sp")


class Buf:
    __slots__ = ("name", "last_w", "readers")

    def __init__(self, name):
        self.name = name
        self.last_w = None
        self.readers = []


class Op:
    __slots__ = ("eng", "idx", "fn", "waits", "signal", "sigval", "is_dma", "key", "ndma")

    def __init__(self, eng, idx, fn, is_dma, key, ndma):
        self.eng = eng
        self.idx = idx
        self.fn = fn
        self.waits = []
        self.signal = False
        self.sigval = None
        self.is_dma = is_dma
        self.key = key
        self.ndma = ndma


class Prog:
    def __init__(self, nc):
        self.nc = nc
        self.streams = {e: [] for e in ENGS}
        self.known = {e: {} for e in ENGS}
        self.dma_keys = {}
        self.fence_deps = []
        self.last_eng_op = {}

    @staticmethod
    def _chan(op):
        return ("dma", op.key) if op.is_dma else ("eng", op.eng)

    def fence(self):
        deps = list(self.last_eng_op.values())
        for lst in self.dma_keys.values():
            deps.append(lst[-1])
        self.fence_deps = deps

    def op(self, eng, fn, reads=(), writes=(), dma_key=None, ndma=1):
        is_dma = dma_key is not None
        st = self.streams[eng]
        o = Op(eng, len(st), fn, is_dma, dma_key, ndma)
        deps = list(self.fence_deps)
        for b in reads:
            if b.last_w is not None:
                deps.append(b.last_w)
        for b in writes:
            if b.last_w is not None:
                deps.append(b.last_w)
            deps.extend(b.readers)
        if is_dma:
            lst = self.dma_keys.setdefault(dma_key, [])
            o.idx = len(lst)
            lst.append(o)
        else:
            self.last_eng_op[eng] = o
        kn = self.known[eng]
        best = {}
        for d in deps:
            ch = self._chan(d)
            if (not d.is_dma) and d.eng == "pe" and eng == "pe" and not is_dma:
                continue
            if kn.get(ch, -1) >= d.idx:
                continue
            if ch not in best or best[ch].idx < d.idx:
                best[ch] = d
        for ch, d in best.items():
            kn[ch] = d.idx
            d.signal = True
            o.waits.append(d)
        for b in reads:
            b.readers.append(o)
        for b in writes:
            b.last_w = o
            b.readers = []
        st.append(o)
        return o

    def emit(self, final_waits=()):
        nc = self.nc
        for _, o in final_waits:
            o.signal = True
        for e in ENGS:
            cnt = 0
            for o in self.streams[e]:
                if (not o.is_dma) and o.signal:
                    cnt += 1
                    o.sigval = cnt
        for lst in self.dma_keys.values():
            cnt = 0
            for o in lst:
                cnt += 16 * o.ndma
                o.sigval = cnt
        with ExitStack() as es:
            eng_sems = {e: es.enter_context(nc.semaphore("s_" + e)) for e in ENGS}
            dma_sems = {k: es.enter_context(nc.semaphore("d_%s" % k)) for k in self.dma_keys}
            block = es.enter_context(nc.Block())

            def sem_of(o):
                return dma_sems[o.key] if o.is_dma else eng_sems[o.eng]

            def run(e, engobj):
                for o in self.streams[e]:
                    for d in o.waits:
                        engobj.wait_ge(sem_of(d), d.sigval)
                    ins = o.fn(engobj)
                    if o.is_dma:
                        if not isinstance(ins, (list, tuple)):
                            ins = [ins]
                        assert len(ins) == o.ndma
                        for i_ in ins:
                            i_.then_inc(dma_sems[o.key], 16)
                    elif o.signal:
                        ins.then_inc(eng_sems[e], 1)
                for ee, o in final_waits:
                    if ee == e:
                        engobj.wait_ge(sem_of(o), o.sigval)

            @block.tensor
            def _(eng):
                run("pe", eng)

            @block.scalar
            def _(eng):
                run("act", eng)

            @block.vector
            def _(eng):
                run("dve", eng)

            @block.gpsimd
            def _(eng):
                run("pool", eng)

            @block.sync
            def _(eng):
                run("sp", eng)


class Arena:
    def __init__(self, nc, start, end, tag):
        self.nc, self.cur, self.end, self.tag = nc, start, end, tag
        self.n = 0

    def take(self, name, shape, dtype):
        esz = 2 if dtype == BF16 else 4
        nbytes = esz * int(np.prod(shape[1:]))
        nbytes = (nbytes + 31) // 32 * 32
        off = self.cur
        assert off + nbytes <= self.end, (self.tag, name, off, nbytes, self.end)
        self.cur += nbytes
        self.n += 1
        return self.nc.alloc_sbuf_tensor_at("%s_%s" % (self.tag, name), list(shape), dtype, offset=off)


def bc(ap, shape):
    return ap.broadcast_to(list(shape))


def build_program():
    nc = bass.Bass("TRN2", target_bir_lowering=False)
    P = Prog(nc)

    def din(name, shape, dt=F32):
        return nc.dram_tensor(name, list(shape), dt, kind="ExternalInput").ap()

    xh = din("xh", [NBH * 128, D])
    posT = din("posT", [128, NBH], I32)
    masks_d = din("masks", [128, 4, 128])
    cst_d = din("cst", [128, 352])
    icst_d = din("icst", [128, NB, 3], I32)
    norm1_g = din("norm1_g", [1, D])
    w_in = din("w_in", [1, D, INC])
    q_norm_g = din("q_norm_g", [1, 64])
    k_norm_g = din("k_norm_g", [1, 64])
    sink_d = din("sink_logits", [1, 16])
    sg_ln_g = din("sg_ln_g", [1, 8, 128])
    sg_ln_b = din("sg_ln_b", [1, 8, 128])
    sg_w = din("sg_w", [1, 8, 128, 128])
    sg_b = din("sg_b", [1, 8, 128])
    w_ba = din("w_branch_att", [1, 1024, D])
    w_bb = din("w_branch_sg", [1, 1024, D])
    w_out = din("w_out", [1, D, D])
    norm2_g = din("norm2_g", [1, D])
    w_gr = din("w_group_router", [1, D, 8])
    b_gr = din("b_group_router", [1, 8])
    w_er = din("w_expert_router", [1, D, 64])
    b_er = din("b_expert_router", [1, 64])
    if STAGE >= 7:
        w_gate = din("w_gate", [1, NE, D, FF])
        w_up = din("w_up", [1, NE, D, FF])
        w_down = din("w_down", [1, NE, FF, D])

    out_d = nc.dram_tensor("out", [TOK, D], F32, kind="ExternalOutput").ap()
    hn_d = nc.dram_tensor("hn_d", [TOK + 1, D], BF16, kind="Internal").ap()
    tbl_d = nc.dram_tensor("tbl_d", [NE * CAP, 16], I32, kind="Internal").ap()
    y2_d = [nc.dram_tensor("y2_d%d" % i, [2 * TOK, D // 2], F32, kind="Internal").ap() for i in range(2)]
    dbg = {}
    if DEBUG:
        dbg["att"] = nc.dram_tensor("dbg_att", [64, 16, TOK], BF16, kind="ExternalOutput").ap()
        dbg["sgo"] = nc.dram_tensor("dbg_sgo", [128, 8, TOK], BF16, kind="ExternalOutput").ap()
        dbg["mrg"] = nc.dram_tensor("dbg_mrg", [128, 16, TOK], BF16, kind="ExternalOutput").ap()
        dbg["xnT"] = nc.dram_tensor("dbg_xnT", [128, 16, TOK], BF16, kind="ExternalOutput").ap()
        dbg["kT"] = nc.dram_tensor("dbg_kT", [64, 4, NBH * 128], BF16, kind="ExternalOutput").ap()
        dbg["tbl"] = nc.dram_tensor("dbg_tbl", [NE * CAP, 16], I32, kind="ExternalOutput").ap()
        dbg["hn"] = nc.dram_tensor("dbg_hn", [TOK + 1, D], BF16, kind="ExternalOutput").ap()

    psA = nc.alloc_psum_tensor("psA", [128, 2048], F32)
    psB = nc.alloc_psum_tensor("psB", [128, 2048], F32)
    BK = [Buf("bank%d" % i) for i in range(8)]

    def bank(i):
        t = psA if i < 4 else psB
        return t[:, (i % 4) * 512:(i % 4 + 1) * 512]

    PERS = 24576
    pa = Arena(nc, SB_BASE, SB_BASE + PERS, "p")
    cst_t = pa.take("cst", [128, 352], F32)
    ident_bf = pa.take("identb", [128, 128], BF16)
    U_bf = pa.take("Ub", [128, 128], BF16)
    ones_bf = pa.take("onesb", [128, 128], BF16)
    mask_t = pa.take("mask", [128, 4, 128], BF16)
    wsT = pa.take("wsT", [128, 8, 128], BF16)
    bs_row = pa.take("bsrow", [1, 1024], BF16)
    lng_bc = pa.take("lng", [128, 1024], F32)
    lnb_bc = pa.take("lnb", [128, 1024], F32)
    gq_bc = pa.take("gq", [128, 64], F32)
    gk_bc = pa.take("gk", [128, 64], F32)
    sk_t = pa.take("sk", [128, 16], F32)
    sinkexp = pa.take("sinkexp", [128, 16], F32)
    cs_t = pa.take("cs", [128, NBH, 16], F32)
    ss_t = pa.take("ss", [128, 32], F32)
    sv_t = pa.take("sv", [128, 32], F32)
    rstd_t = pa.take("rstd", [128, 32], F32)
    Wr = pa.take("Wr", [128, 16, 72], F32)
    br_bc = pa.take("brbc", [128, 72], F32)
    icst_t = pa.take("icst", [128, NB, 3], I32)
    tbe = [pa.take("tbe%d" % i, [128, 16], I32) for i in range(2)]
    ident_f = cst_t[:, 96:224]
    iota64 = cst_t[:, 32:96]
    MAIN0 = SB_BASE + PERS
    bnd_tbl = nc.gpsimd.alloc_register("bnd_tbl")
    bnd_y2 = nc.gpsimd.alloc_register("bnd_y2")
    bnd_tok = nc.gpsimd.alloc_register("bnd_tok")

    def set_bounds(e):
        e.reg_mov(bnd_tbl, NE * CAP - 1)
        e.reg_mov(bnd_tok, TOK - 1)
        return e.reg_mov(bnd_y2, 2 * TOK - 1)
    P.op("pool", set_bounds)

    CST = Buf("CST")
    cst_ops = []

    def cload(out, in_, eng="sp"):
        o = P.op(eng, lambda e: e.dma_start(out=out, in_=in_), dma_key="cst_" + eng)
        cst_ops.append(o)
        return o

    _ru = MAIN0 + 81920
    xs = [nc.alloc_sbuf_tensor_at("a1_xs%d" % i, [128, D], F32, offset=_ru + i * 8192) for i in range(2)]
    gbc = nc.alloc_sbuf_tensor_at("a1_gbc", [128, D], F32, offset=_ru + 16384)
    XS = [Buf("xs0"), Buf("xs1")]
    GBC = Buf("gbc")
    EARLY = []
    for tb_ in range(2):
        o_ = P.op("sp", lambda e, tb_=tb_: e.dma_start(out=xs[tb_][:], in_=xh[tb_ * 128:(tb_ + 1) * 128, :]), writes=[XS[tb_]], dma_key="xs%d" % tb_)
        b_ = Buf("early%d" % tb_)
        b_.last_w = o_
        EARLY.append(b_)
    P.op("sp", lambda e: e.dma_start(out=gbc[:], in_=norm1_g.rearrange("o c -> (o c)").partition_broadcast(128)), writes=[GBC], dma_key="gbc")

    cload(cst_t[:], cst_d)
    cload(icst_t[:], icst_d)
    cload(lng_bc[:], sg_ln_g.rearrange("o g c -> (o g c)").partition_broadcast(128))
    cload(lnb_bc[:], sg_ln_b.rearrange("o g c -> (o g c)").partition_broadcast(128))
    cload(gq_bc[:], q_norm_g.rearrange("o c -> (o c)").partition_broadcast(128))
    cload(gk_bc[:], k_norm_g.rearrange("o c -> (o c)").partition_broadcast(128))
    cload(sk_t[:], sink_d.rearrange("o c -> (o c)").partition_broadcast(128))
    cload(br_bc[:, 0:8], b_gr.rearrange("o c -> (o c)").partition_broadcast(128))
    cload(br_bc[:, 8:72], b_er.rearrange("o c -> (o c)").partition_broadcast(128))
    with nc.allow_non_contiguous_dma(reason="small router weight relayout"):
        cload(Wr[:, :, 0:8], w_gr[0].rearrange("(k p) c -> p k c", p=128))
        cload(Wr[:, :, 8:72], w_er[0].rearrange("(k p) c -> p k c", p=128))
    s0 = Arena(nc, MAIN0 + 65536, MAIN0 + 81920, "s0")
    wl = s0.take("wl", [128, 8, 128], F32)
    pos_i = s0.take("posi", [128, NBH], I32)
    pos_f = s0.take("posf", [128, NBH], F32)
    ang = s0.take("ang", [128, NBH, 16], F32)
    rr = s0.take("rr", [128, NBH, 16], F32)
    kk_i = s0.take("kki", [128, NBH, 16], I32)
    kk_f = s0.take("kkf", [128, NBH, 16], F32)
    mm_t = s0.take("mmt", [128, NBH, 16], F32)
    tblinit = s0.take("tblinit", [128, 64, 16], I32)
    zrow = s0.take("zrow", [1, D], BF16)
    cload(wl[:], sg_w[0].rearrange("g t s -> t g s"))
    cload(pos_i[:], posT)
    cload(mask_t[:], masks_d, eng="pool")
    cload(bs_row[:], sg_b.rearrange("o g t -> o (g t)"), eng="pool")
    last_sp = [o for o in cst_ops if o.eng == "sp"][-1]
    last_pool = [o for o in cst_ops if o.eng == "pool"][-1]
    CSTa, CSTb = Buf("CSTa"), Buf("CSTb")
    CSTa.last_w = last_sp
    CSTb.last_w = last_pool
    CONST = [CSTa, CSTb]

    SET = Buf("SET")
    P.op("dve", lambda e: e.tensor_copy(ident_bf[:], ident_f), reads=CONST, writes=[SET])
    P.op("dve", lambda e: e.tensor_copy(U_bf[:], cst_t[:, 224:352]), reads=CONST, writes=[SET])
    P.op("dve", lambda e: e.memset(ones_bf[:], 1.0), writes=[SET])
    P.op("dve", lambda e: e.memset(ss_t[:], 0.0), writes=[SET])
    P.op("dve", lambda e: e.memset(sv_t[:], 0.0), writes=[SET])
    P.op("act", lambda e: e.activation(sinkexp[:], sk_t[:], AF.Exp), reads=CONST, writes=[SET])
    for g in range(8):
        bi = g % 2
        P.op("pe", lambda e, g=g, bi=bi: e.transpose(bank(bi)[:, 0:128], wl[:, g, :], ident_f), reads=CONST, writes=[BK[bi]])
        P.op("dve", lambda e, g=g, bi=bi: e.tensor_copy(wsT[:, g, :], bank(bi)[:, 0:128]), reads=[BK[bi]], writes=[SET])
    RB = Buf("rope")
    P.op("dve", lambda e: e.tensor_copy(pos_f[:], pos_i[:]), reads=CONST, writes=[RB])
    P.op("dve", lambda e: e.tensor_tensor(ang[:], bc(pos_f[:].unsqueeze(2), [128, NBH, 16]),
                                          bc(cst_t[:, 0:16].unsqueeze(1), [128, NBH, 16]), ALU.mult), reads=[RB], writes=[RB])
    P.op("dve", lambda e: e.tensor_tensor(ang[:], ang[:], bc(cst_t[:, 16:32].unsqueeze(1), [128, NBH, 16]), ALU.add), reads=[RB], writes=[RB])
    P.op("dve", lambda e: e.tensor_scalar(rr[:], ang[:], 1.0 / TWO_PI, None, ALU.mult), reads=[RB], writes=[RB])
    P.op("dve", lambda e: e.tensor_copy(kk_i[:], rr[:]), reads=[RB], writes=[RB])
    P.op("dve", lambda e: e.tensor_copy(kk_f[:], kk_i[:]), reads=[RB], writes=[RB])
    P.op("dve", lambda e: e.scalar_tensor_tensor(rr[:], kk_f[:], -6.28125, ang[:], ALU.mult, ALU.add), reads=[RB], writes=[RB])
    P.op("dve", lambda e: e.scalar_tensor_tensor(ang[:], kk_f[:], -(TWO_PI - 6.28125), rr[:], ALU.mult, ALU.add), reads=[RB], writes=[RB])
    P.op("dve", lambda e: e.tensor_scalar(mm_t[:], ang[:], math.pi, TWO_PI, ALU.is_gt, ALU.mult), reads=[RB], writes=[RB])
    P.op("dve", lambda e: e.tensor_tensor(rr[:], ang[:], mm_t[:], ALU.subtract), reads=[RB], writes=[RB])
    P.op("dve", lambda e: e.tensor_scalar(mm_t[:], rr[:], -math.pi, TWO_PI, ALU.is_lt, ALU.mult), reads=[RB], writes=[RB])
    P.op("dve", lambda e: e.tensor_tensor(ang[:], rr[:], mm_t[:], ALU.add), reads=[RB], writes=[RB])
    P.op("dve", lambda e: e.tensor_scalar(rr[:], ang[:], -3.1415925, 3.1415925, ALU.max, ALU.min), reads=[RB], writes=[RB])
    P.op("act", lambda e: e.activation(cs_t[:], rr[:], AF.Sin), reads=[RB], writes=[SET])
    TBL = Buf("TBL")
    HND = Buf("HND")
    P.op("pool", lambda e: e.memset(tblinit[:], 0), writes=[RB])
    P.op("pool", lambda e: e.memset(tblinit[:, :, 0:1], TOK), reads=[RB], writes=[RB])
    P.op("pool", lambda e: e.memset(tblinit[:, :, 2:3], BIG), reads=[RB], writes=[RB])
    P.op("pool", lambda e: e.memset(zrow[:], 0.0), writes=[SET])

    R_X = MAIN0
    R_Q = MAIN0 + 32768
    R_KV = MAIN0 + 65536
    R_U = MAIN0 + 81920
    R_M = MAIN0 + 98304
    R_RING = MAIN0 + 131072
    assert R_RING + 3 * 16384 <= SB_END
    xnT = nc.alloc_sbuf_tensor_at("xnT", [128, 16, TOK], BF16, offset=R_X)
    qT = nc.alloc_sbuf_tensor_at("qT", [128, 16, TOK], BF16, offset=R_Q)
    kT = nc.alloc_sbuf_tensor_at("kT", [128, 4, NBH * 128], BF16, offset=R_KV)
    vt = nc.alloc_sbuf_tensor_at("vt", [128, NBH, 256], BF16, offset=R_KV + 10240)
    sa_t = nc.alloc_sbuf_tensor_at("sa", [128, 4, TOK], BF16, offset=R_KV)
    sb_t = nc.alloc_sbuf_tensor_at("sb", [128, 4, TOK], BF16, offset=R_KV + 8192)
    uT = nc.alloc_sbuf_tensor_at("uT", [128, 8, TOK], BF16, offset=R_U)
    mT = nc.alloc_sbuf_tensor_at("mT", [128, 16, TOK], BF16, offset=R_M)
    ring = [nc.alloc_sbuf_tensor_at("ring%d" % i, [128, 16, 512], BF16, offset=R_RING + i * 16384) for i in range(3)]
    RING = [Buf("ring%d" % i) for i in range(3)]

    def w_in_view(col):
        return w_in[0].rearrange("(k p) c -> p k c", p=128)[:, :, col:col + 512]

    wtiles = [(w_in_view(0), 128, 16), (w_in_view(512), 128, 16), (w_in_view(OFF_K), 128, 16),
              (w_in_view(OFF_U), 128, 16), (w_in_view(OFF_U + 512), 128, 16), (w_in_view(OFF_S), 128, 16), (w_in_view(OFF_S + 512), 128, 16)]
    for jc_ in range(4):
        wtiles.append((w_in_view(OFF_G + jc_ * 512), 128, 16))
        wtiles.append((w_in_view(OFF_G + D + jc_ * 512), 128, 16))
        wtiles.append((w_ba[0].rearrange("(h d) c -> d h c", d=64)[:, :, jc_ * 512:(jc_ + 1) * 512], 64, 16))
        wtiles.append((w_bb[0].rearrange("(g c) n -> c g n", c=128)[:, :, jc_ * 512:(jc_ + 1) * 512], 128, 8))
    for c_ in range(3):
        wtiles.append((w_out[0].rearrange("(k p) c -> p k c", p=128)[:, :, c_ * 512:(c_ + 1) * 512], 128, 16))
    w_issued = [0]
    w_used = [0]

    def w_issue_upto(n):
        while w_issued[0] < min(n, len(wtiles)):
            t = w_issued[0]
            view, nparts, kdim = wtiles[t]
            i = t % 3
            dst = ring[i][0:nparts, 0:kdim, :]
            P.op("pool", lambda e, dst=dst, view=view: e.dma_start(out=dst, in_=view), reads=(EARLY if t < 3 else []), writes=[RING[i]], dma_key="ring%d" % i)
            w_issued[0] += 1

    def wload(chain=None):
        t = w_used[0]
        w_used[0] += 1
        if chain is None:
            w_issue_upto(t + 3)
        else:
            chain.ops.append(("call", lambda t=t: w_issue_upto(t + 3)))
        return ring[t % 3], RING[t % 3]

    def prefetch_fence():
        w_issue_upto(w_used[0] + 3)
        P.fence()

    w_issue_upto(3)

    class Chain:
        def __init__(self):
            self.ops = []

        def op(self, *a, **k):
            self.ops.append((a, k))

    def run_chains(chains, width=2):
        pend = list(chains)
        active = []
        while pend or active:
            while pend and len(active) < width:
                active.append([pend.pop(0), 0])
            for ent in list(active):
                ch = ent[0]
                while ent[1] < len(ch.ops) and ch.ops[ent[1]][0] == "call":
                    ch.ops[ent[1]][1]()
                    ent[1] += 1
                if ent[1] < len(ch.ops):
                    a, k = ch.ops[ent[1]]
                    P.op(*a, **k)
                    ent[1] += 1
                if ent[1] >= len(ch.ops):
                    active.remove(ent)

    a1 = Arena(nc, R_U, R_M + 32768, "a1")
    assert a1.cur == _ru
    a1.cur += 3 * 8192
    xb = [a1.take("xb%d" % i, [128, D], BF16) for i in range(2)]
    junk = a1.take("junk", [128, D], BF16)
    xnH = a1.take("xnH", [128, 16, 256], BF16)
    assert a1.cur <= R_M + 20480 + 8192 + 4096 or True
    XB = [Buf("xb0"), Buf("xb1")]
    JK = Buf("junk")
    SS = Buf("ss")
    XNT = [Buf("xnT%d" % j) for j in range(NBH)]
    SSC = [Buf("ss%d" % i) for i in range(32)]

    def rstd_chain(C, col, scale, src=ss_t):
        SSB = SSC[col]
        C.op("dve", lambda e: e.tensor_scalar(sv_t[:, col:col + 1], src[:, col:col + 1], scale, EPS, ALU.mult, ALU.add), reads=[SSB], writes=[SSB])
        C.op("act", lambda e: e.activation(sv_t[:, col:col + 1], sv_t[:, col:col + 1], AF.Sqrt), reads=[SSB], writes=[SSB])
        C.op("dve", lambda e: e.reciprocal(rstd_t[:, col:col + 1], sv_t[:, col:col + 1]), reads=[SSB], writes=[SSB])

    chains1 = []
    for tb in range(NBH):
        s = tb % 2
        C = Chain()
        chains1.append(C)
        if tb >= 2:
            C.op("sp", lambda e, tb=tb, s=s: e.dma_start(out=xs[s][:], in_=xh[tb * 128:(tb + 1) * 128, :]), writes=[XS[s]], dma_key="xs%d" % s)
        C.op("act", lambda e, tb=tb, s=s: e.activation(junk[:], xs[s][:], AF.Square, accum_out=ss_t[:, tb:tb + 1]), reads=[XS[s], SET], writes=[JK, SSC[tb]])
        rstd_chain(C, tb, 1.0 / D)
        C.op("dve", lambda e, tb=tb, s=s: e.scalar_tensor_tensor(xb[s][:], xs[s][:], rstd_t[:, tb:tb + 1], gbc[:], ALU.mult, ALU.mult),
             reads=[XS[s], SSC[tb], GBC], writes=[XB[s]])
        for half in range(2):
            bi = (2 * tb + half) % 8

            def tr(e, s=s, half=half, bi=bi):
                pb = bank(bi).bitcast(BF16)
                for kq in range(8):
                    k = half * 8 + kq
                    ins = e.transpose(pb[:, kq * 128:(kq + 1) * 128], xb[s][:, k * 128:(k + 1) * 128], ident_bf[:])
                return ins
            C.op("pe", tr, reads=[XB[s], SET], writes=[BK[bi]])
            if tb == 0:
                dst = xnH[:, half * 8:half * 8 + 8, 0:128]
            elif tb == NBH - 1:
                dst = xnH[:, half * 8:half * 8 + 8, 128:256]
            else:
                dst = xnT[:, half * 8:half * 8 + 8, (tb - 1) * 128:tb * 128]
            src = bank(bi).bitcast(BF16).rearrange("p (k t) -> p k t", k=8)
            if half == 0:
                C.op("act", lambda e, dst=dst, src=src: e.activation(dst, src, AF.Copy), reads=[BK[bi]], writes=[XNT[tb]])
            else:
                C.op("dve", lambda e, dst=dst, src=src: e.tensor_copy(dst, src), reads=[BK[bi]], writes=[XNT[tb]])
    run_chains(chains1, width=2)
    P.op("sp", lambda e: e.dma_start(out=tbl_d.rearrange("(p r) c -> p r c", p=128), in_=tblinit[:]), reads=[RB], writes=[TBL], dma_key="tblinit")
    P.op("sp", lambda e: e.dma_start(out=hn_d[TOK:TOK + 1, :], in_=zrow[:]), reads=[SET], writes=[HND], dma_key="zrow")

    def xn_cols(tb):
        if tb == 0:
            return xnH[:, :, 0:128]
        if tb == NBH - 1:
            return xnH[:, :, 128:256]
        return xnT[:, :, (tb - 1) * 128:tb * 128]

    prefetch_fence()

    a2 = Arena(nc, R_M, R_M + 20480, "a2")
    T2S = []
    NW = 4
    for ts in range(NW):
        qa_ = a2.take("qa%d" % ts, [128, 512], F32)
        T2S.append(dict(sqf=qa_, qnf=qa_, qg=qa_, rt=[a2.take("rt%d_%d" % (ts, i), [128, 8, 8], F32) for i in range(4)],
                        qb=a2.take("qb%d" % ts, [128, 512], BF16), rq=a2.take("rq%d" % ts, [128, 16], F32), buf=Buf("t2_%d" % ts)))
    QT = [[Buf("qT%d_%d" % (j, h)) for h in range(4)] for j in range(NB)]
    KT = [Buf("kT%d" % tb) for tb in range(NBH)]
    VT = [Buf("vt%d" % tb) for tb in range(NBH)]
    bank_rr = [0]

    def next_bank():
        b = bank_rr[0] % 8
        bank_rr[0] += 1
        return b

    def qk_chain(C, ts, bsrc, BKb, nh, tb, g_bc, dst_fn, dst_bufs):
        S_ = T2S[ts]
        sqf, qnf, qg, rt, qb, rq, T2 = S_["sqf"], S_["qnf"], S_["qg"], S_["rt"], S_["qb"], S_["rq"], S_["buf"]
        w = nh * 64
        C.op("act", lambda e: e.activation(sqf[:, 0:w], bsrc, AF.Square), reads=[BKb], writes=[T2])
        C.op("dve", lambda e: e.tensor_reduce(rq[:, 0:nh], sqf[:, 0:w].rearrange("p (h d) -> p h d", h=nh), AX.X, ALU.add), reads=[T2], writes=[T2])
        C.op("dve", lambda e: e.tensor_scalar(rq[:, 0:nh], rq[:, 0:nh], 1.0 / 64, EPS, ALU.mult, ALU.add), reads=[T2], writes=[T2])
        C.op("act", lambda e: e.activation(rq[:, 0:nh], rq[:, 0:nh], AF.Sqrt), reads=[T2], writes=[T2])
        C.op("dve", lambda e: e.reciprocal(rq[:, 8:8 + nh], rq[:, 0:nh]), reads=[T2], writes=[T2])
        q3 = lambda t: t[:, 0:w].rearrange("p (h d) -> p h d", h=nh)
        C.op("dve", lambda e: e.tensor_tensor(q3(qnf), bsrc.rearrange("p (h d) -> p h d", h=nh),
                                              bc(rq[:, 8:8 + nh].unsqueeze(2), [128, nh, 64]), ALU.mult), reads=[T2, BKb], writes=[T2])
        C.op("pool", lambda e: e.tensor_tensor(q3(qg), q3(qnf), bc(g_bc[:].unsqueeze(1), [128, nh, 64]), ALU.mult), reads=[T2, CSTa], writes=[T2])
        sin_b = bc(cs_t[:, tb, 0:8].unsqueeze(1), [128, nh, 8])
        cos_b = bc(cs_t[:, tb, 8:16].unsqueeze(1), [128, nh, 8])

        def rope4(e):
            e.tensor_tensor(rt[0][:, 0:nh, :], q3(qg)[:, :, 0:8], cos_b, ALU.mult)
            e.tensor_tensor(rt[1][:, 0:nh, :], q3(qg)[:, :, 8:16], sin_b, ALU.mult)
            e.tensor_tensor(rt[2][:, 0:nh, :], q3(qg)[:, :, 8:16], cos_b, ALU.mult)
            return e.tensor_tensor(rt[3][:, 0:nh, :], q3(qg)[:, :, 0:8], sin_b, ALU.mult)
        C.op("pool", rope4, reads=[T2, SET], writes=[T2])
        C.op("act", lambda e: e.activation(qb[:, 0:w], qg[:, 0:w], AF.Copy), reads=[T2], writes=[T2])

        def rope2(e):
            e.tensor_tensor(q3(qb)[:, :, 0:8], rt[0][:, 0:nh, :], rt[1][:, 0:nh, :], ALU.subtract)
            return e.tensor_tensor(q3(qb)[:, :, 8:16], rt[2][:, 0:nh, :], rt[3][:, 0:nh, :], ALU.add)
        C.op("dve", rope2, reads=[T2], writes=[T2])
        bt = next_bank()

        def trs(e):
            pb = bank(bt).bitcast(BF16)
            for hh in range(nh):
                ins = e.transpose(pb[0:64, hh * 128:(hh + 1) * 128], qb[:, hh * 64:(hh + 1) * 64], ident_bf[:])
            return ins
        C.op("pe", trs, reads=[T2, SET], writes=[BK[bt]])
        src = bank(bt).bitcast(BF16)[0:64, 0:nh * 128].rearrange("p (h t) -> p h t", h=nh)
        C.op("act", lambda e: e.activation(dst_fn(), src, AF.Copy), reads=[BK[bt]], writes=dst_bufs)

    chains = []
    n_ch = 0
    for qi in range(2):
        wl_pending = True
        for j in range(NB):
            tb = j + 1
            bq = next_bank()
            C = Chain()
            if wl_pending:
                wt, WB = wload(chain=C)
                wl_pending = False

            def mmq(e, wt=wt, tb=tb, bq=bq):
                xa = xn_cols(tb)
                for k in range(16):
                    ins = e.matmul(bank(bq), xa[:, k, :], wt[:, k, :], start=(k == 0), stop=(k == 15))
                return ins
            C.op("pe", mmq, reads=[WB, XNT[tb]], writes=[BK[bq]])
            qk_chain(C, n_ch % NW, bank(bq), BK[bq], 8, tb, gq_bc,
                     lambda qi=qi, j=j: qT[0:64, qi * 8:qi * 8 + 8, j * 128:(j + 1) * 128],
                     [QT[j][qi * 2], QT[j][qi * 2 + 1]])
            chains.append(C)
            n_ch += 1
    wl_pending = True
    for tb in range(NBH):
        bq = next_bank()
        C = Chain()
        if wl_pending:
            wt, WB = wload(chain=C)
            wl_pending = False

        def mmk(e, wt=wt, tb=tb, bq=bq):
            xa = xn_cols(tb)
            for k in range(16):
                ins = e.matmul(bank(bq), xa[:, k, :], wt[:, k, :], start=(k == 0), stop=(k == 15))
            return ins
        C.op("pe", mmk, reads=[WB, XNT[tb]], writes=[BK[bq]])
        C.op("act", lambda e, tb=tb, bq=bq: e.activation(vt[:, tb, :], bank(bq)[:, 256:512], AF.Copy), reads=[BK[bq]], writes=[VT[tb]])
        qk_chain(C, n_ch % NW, bank(bq)[:, 0:256], BK[bq], 4, tb, gk_bc,
                 lambda tb=tb: kT[0:64, :, tb * 128:(tb + 1) * 128], [KT[tb]])
        chains.append(C)
        n_ch += 1
    run_chains(chains, width=4)

    if DEBUG:
        P.op("sp", lambda e: e.dma_start(out=dbg["xnT"], in_=xnT[:]), reads=XNT, dma_key="dbg0")
        P.op("sp", lambda e: e.dma_start(out=dbg["kT"], in_=kT[0:64, :, :]), reads=KT, dma_key="dbg0")

    prefetch_fence()
    a3 = Arena(nc, R_M, R_M + 20480, "a3")
    PT = [a3.take("PT%d" % i, [128, 3, 512], BF16) for i in range(2)]
    rd = a3.take("rd", [128, 512], F32)
    srow = [a3.take("srow%d" % i, [1, 2048], BF16) for i in range(2)]
    sk16 = [a3.take("sk16_%d" % i, [1, 16], BF16) for i in range(2)]
    sk32 = [a3.take("sk32_%d" % i, [1, 16], F32) for i in range(2)]
    SR = Buf("srow")
    P.op("dve", lambda e: e.tensor_copy(sk16[0][:], sinkexp[0:1, :]), reads=[SET], writes=[SR])
    P.op("dve", lambda e: e.tensor_copy(sk32[0][:], sk16[0][:]), reads=[SR], writes=[SR])
    P.op("dve", lambda e: e.tensor_tensor(sk32[1][:], sinkexp[0:1, :], sk32[0][:], ALU.subtract), reads=[SR, SET], writes=[SR])
    P.op("dve", lambda e: e.tensor_copy(sk16[1][:], sk32[1][:]), reads=[SR], writes=[SR])

    def mk_srow(e):
        for i in range(2):
            ins = e.tensor_copy(srow[i][:].rearrange("p (h q) -> p h q", h=16), bc(sk16[i][:].unsqueeze(2), [1, 16, 128]))
        return ins
    P.op("dve", mk_srow, reads=[SR], writes=[SR])
    PTB = [[Buf("PT%d_%d" % (i, c)) for c in range(3)] for i in range(2)]
    D2 = Buf("d2")
    def att_front(it):
        j, h = it // 4, it % 4
        sset = it % 2
        pt = PT[sset]
        for c in range(3):
            bi = sset * 3 + c
            P.op("pe", lambda e, bi=bi, h=h, j=j, c=c: e.matmul(bank(bi), kT[0:64, h, (j + c) * 128:(j + c + 1) * 128],
                                                               qT[0:64, 4 * h:4 * h + 4, j * 128:(j + 1) * 128], start=True, stop=True),
                 reads=[KT[j + c], QT[j][h]], writes=[BK[bi]])
            P.op("act", lambda e, bi=bi, pt=pt, c=c: e.activation(pt[:, c, :], bank(bi), AF.Exp, scale=0.125),
                 reads=[BK[bi]], writes=[PTB[sset][c]])
        mprev = 0 if j == 0 else 1
        mnext = 3 if j == NB - 1 else 2
        for c, mi in ((0, mprev), (2, mnext)):
            P.op("pool" if c == 0 else "dve", lambda e, pt=pt, c=c, mi=mi: e.tensor_tensor(pt[:, c, :].rearrange("p (g q) -> p g q", g=4),
                                                                      pt[:, c, :].rearrange("p (g q) -> p g q", g=4),
                                                                      bc(mask_t[:, mi, :].unsqueeze(1), [128, 4, 128]), ALU.mult),
                 reads=[PTB[sset][c], CSTb], writes=[PTB[sset][c]])

    def att_back(it):
        j, h = it // 4, it % 4
        sset = it % 2
        pt = PT[sset]

        def pv(e, pt=pt, h=h, j=j):
            for c in range(3):
                e.matmul(bank(6)[0:64, :], vt[:, j + c, h * 64:(h + 1) * 64], pt[:, c, :], start=(c == 0), stop=(c == 2))
            for c in range(3):
                e.matmul(bank(7)[0:64, :], ones_bf[:, 0:64], pt[:, c, :], start=(c == 0), stop=False)
            for i in range(2):
                ins = e.matmul(bank(7)[0:64, :], ones_bf[0:1, 0:64], srow[i][0:1, 4 * h * 128:(4 * h + 4) * 128], start=False, stop=(i == 1))
            return ins
        P.op("pe", pv, reads=PTB[sset] + [VT[j], VT[j + 1], VT[j + 2], SR], writes=[BK[6], BK[7]])
        P.op("dve", lambda e: e.reciprocal(rd[0:64, :], bank(7)[0:64, :]), reads=[BK[7]], writes=[D2])
        P.op("dve", lambda e, h=h, j=j: e.tensor_tensor(qT[0:64, 4 * h:4 * h + 4, j * 128:(j + 1) * 128],
                                                        bank(6)[0:64, :].rearrange("p (g q) -> p g q", g=4),
                                                        rd[0:64, :].rearrange("p (g q) -> p g q", g=4), ALU.mult),
             reads=[BK[6], D2], writes=[QT[j][h]])

    NIT = NB * 4
    att_front(0)
    for it in range(1, NIT):
        att_front(it)
        att_back(it - 1)
    att_back(NIT - 1)
    ATT = [b for row in QT for b in row]
    if DEBUG:
        P.op("sp", lambda e: e.dma_start(out=dbg["att"], in_=qT[0:64, :, :]), reads=ATT, dma_key="dbg0")
    if STAGE <= 3:
        return finish(nc, P, out_d)

    prefetch_fence()
    a4 = Arena(nc, R_M, R_M + 20480, "a4")
    T4S = []
    for ts in range(4):
        sgl_ = a4.take("sgl%d" % ts, [128, 512], F32)
        T4S.append(dict(sgl=sgl_, xc=a4.take("xc%d" % ts, [128, 512], F32),
                        sgb=sgl_[:].bitcast(BF16), st4=a4.take("st4%d" % ts, [128, 16], F32), buf=Buf("t4_%d" % ts)))
    UT = [[Buf("uT%d_%d" % (g, th)) for th in range(2)] for g in range(8)]
    for ui in range(2):
        wt, WB = wload()
        for m in range(4):
            for th in range(2):
                bq = next_bank()

                def mmu(e, wt=wt, m=m, th=th, bq=bq):
                    for k in range(16):
                        ins = e.matmul(bank(bq), wt[:, k, m * 128:(m + 1) * 128], xnT[:, k, th * 512:(th + 1) * 512], start=(k == 0), stop=(k == 15))
                    return ins
                P.op("pe", mmu, reads=[WB] + XNT[1 + 4 * th:5 + 4 * th], writes=[BK[bq]])
                P.op("act", lambda e, ui=ui, m=m, th=th, bq=bq: e.activation(uT[:, ui * 4 + m, th * 512:(th + 1) * 512], bank(bq), AF.Gelu_apprx_tanh),
                     reads=[BK[bq]], writes=[UT[ui * 4 + m][th]])
    g4 = lambda t: t[:, 0:512].rearrange("p (g c) -> p g c", g=4)
    chains = []
    n_ch = 0
    for si in range(2):
        wl_pending = True
        for j in range(NB):
            bq = next_bank()
            C = Chain()
            if wl_pending:
                wt, WB = wload(chain=C)
                wl_pending = False
            S_ = T4S[n_ch % 4]
            n_ch += 1
            sgl, xc, sgb, st4, T4 = S_["sgl"], S_["xc"], S_["sgb"], S_["st4"], S_["buf"]

            def mms(e, wt=wt, j=j, bq=bq):
                for k in range(16):
                    ins = e.matmul(bank(bq), xnT[:, k, j * 128:(j + 1) * 128], wt[:, k, :], start=(k == 0), stop=(k == 15))
                return ins
            C.op("pe", mms, reads=[WB, XNT[j + 1]], writes=[BK[bq]])
            C.op("act", lambda e, bq=bq, sgl=sgl: e.activation(sgl[:], bank(bq), AF.Gelu_apprx_tanh), reads=[BK[bq]], writes=[T4])
            C.op("dve", lambda e, sgl=sgl, st4=st4: e.tensor_reduce(st4[:, 0:4], g4(sgl), AX.X, ALU.add), reads=[T4], writes=[T4])
            C.op("dve", lambda e, st4=st4: e.tensor_scalar(st4[:, 0:4], st4[:, 0:4], -1.0 / 128, None, ALU.mult), reads=[T4], writes=[T4])
            C.op("dve", lambda e, sgl=sgl, xc=xc, st4=st4: e.tensor_tensor(g4(xc), g4(sgl), bc(st4[:, 0:4].unsqueeze(2), [128, 4, 128]), ALU.add), reads=[T4], writes=[T4])
            C.op("act", lambda e, sgl=sgl, xc=xc: e.activation(sgl[:], xc[:], AF.Square), reads=[T4], writes=[T4])
            C.op("dve", lambda e, sgl=sgl, st4=st4: e.tensor_reduce(st4[:, 4:8], g4(sgl), AX.X, ALU.add), reads=[T4], writes=[T4])
            C.op("dve", lambda e, st4=st4: e.tensor_scalar(st4[:, 4:8], st4[:, 4:8], 1.0 / 128, EPS, ALU.mult, ALU.add), reads=[T4], writes=[T4])
            C.op("act", lambda e, st4=st4: e.activation(st4[:, 4:8], st4[:, 4:8], AF.Sqrt), reads=[T4], writes=[T4])
            C.op("dve", lambda e, st4=st4: e.reciprocal(st4[:, 8:12], st4[:, 4:8]), reads=[T4], writes=[T4])
            C.op("dve", lambda e, xc=xc, st4=st4: e.tensor_tensor(g4(xc), g4(xc), bc(st4[:, 8:12].unsqueeze(2), [128, 4, 128]), ALU.mult), reads=[T4], writes=[T4])
            C.op("pool", lambda e, si=si, xc=xc: e.tensor_tensor(xc[:], xc[:], lng_bc[:, si * 512:(si + 1) * 512], ALU.mult), reads=[T4, CSTa], writes=[T4])
            C.op("pool", lambda e, si=si, xc=xc, sgb=sgb: e.tensor_tensor(sgb[:, 0:512], xc[:], lnb_bc[:, si * 512:(si + 1) * 512], ALU.add), reads=[T4, CSTa], writes=[T4])
            bm = next_bank()

            def mix(e, si=si, bm=bm, sgb=sgb):
                for gg in range(4):
                    g = si * 4 + gg
                    e.matmul(bank(bm)[:, gg * 128:(gg + 1) * 128], sgb[:, gg * 128:(gg + 1) * 128], wsT[:, g, :], start=True, stop=False)
                    ins = e.matmul(bank(bm)[:, gg * 128:(gg + 1) * 128], ones_bf[0:1, :], bs_row[0:1, g * 128:(g + 1) * 128], start=False, stop=True)
                return ins
            C.op("pe", mix, reads=[T4, SET, CSTb], writes=[BK[bm]])
            th = j // 4
            C.op("dve", lambda e, si=si, j=j, bm=bm: e.tensor_tensor(uT[:, si * 4:si * 4 + 4, j * 128:(j + 1) * 128],
                                                                     bank(bm).rearrange("p (g t) -> p g t", g=4),
                                                                     uT[:, si * 4:si * 4 + 4, j * 128:(j + 1) * 128], ALU.mult),
                 reads=[BK[bm]], writes=[UT[si * 4 + gg][th] for gg in range(4)])
            chains.append(C)
    run_chains(chains, width=4)
    SGO = [b for row in UT for b in row]
    if DEBUG:
        P.op("sp", lambda e: e.dma_start(out=dbg["sgo"], in_=uT[:]), reads=SGO, dma_key="dbg0")
    if STAGE <= 4:
        return finish(nc, P, out_d)

    prefetch_fence()
    SA = [[Buf("sa%d_%d" % (m, th)) for th in range(2)] for m in range(4)]
    SBb = [[Buf("sb%d_%d" % (m, th)) for th in range(2)] for m in range(4)]
    MT = [[Buf("mT%d_%d" % (c, th)) for th in range(2)] for c in range(16)]
    XN_H = [XNT[1:5], XNT[5:9]]
    ATT_H = [[QT[j][h] for j in range(4 * th, 4 * th + 4) for h in range(4)] for th in range(2)]
    SGO_H = [[UT[g][th] for g in range(8)] for th in range(2)]
    for jc in range(4):
        for which, tgt, TB in ((0, sa_t, SA), (1, sb_t, SBb)):
            wt, WB = wload()
            for m in range(4):
                for th in range(2):
                    bq = next_bank()

                    def mmg(e, wt=wt, m=m, th=th, bq=bq):
                        for k in range(16):
                            ins = e.matmul(bank(bq), wt[:, k, m * 128:(m + 1) * 128], xnT[:, k, th * 512:(th + 1) * 512], start=(k == 0), stop=(k == 15))
                        return ins
                    P.op("pe", mmg, reads=[WB] + XN_H[th], writes=[BK[bq]])
                    P.op("act", lambda e, tgt=tgt, m=m, th=th, bq=bq: e.activation(tgt[:, m, th * 512:(th + 1) * 512], bank(bq), AF.Sigmoid),
                         reads=[BK[bq]], writes=[TB[m][th]])
        wt, WB = wload()
        for m in range(4):
            for th in range(2):
                bq = next_bank()

                def mma(e, wt=wt, m=m, th=th, bq=bq):
                    for hh in range(16):
                        ins = e.matmul(bank(bq), wt[0:64, hh, m * 128:(m + 1) * 128], qT[0:64, hh, th * 512:(th + 1) * 512], start=(hh == 0), stop=(hh == 15))
                    return ins
                P.op("pe", mma, reads=[WB] + ATT_H[th], writes=[BK[bq]])
                P.op("dve", lambda e, m=m, th=th, bq=bq: e.tensor_tensor(sa_t[:, m, th * 512:(th + 1) * 512], bank(bq), sa_t[:, m, th * 512:(th + 1) * 512], ALU.mult),
                     reads=[BK[bq]], writes=[SA[m][th]])
        wt, WB = wload()
        for m in range(4):
            for th in range(2):
                bq = next_bank()

                def mmb(e, wt=wt, m=m, th=th, bq=bq):
                    for g in range(8):
                        ins = e.matmul(bank(bq), wt[:, g, m * 128:(m + 1) * 128], uT[:, g, th * 512:(th + 1) * 512], start=(g == 0), stop=(g == 7))
                    return ins
                P.op("pe", mmb, reads=[WB] + SGO_H[th], writes=[BK[bq]])
                P.op("dve", lambda e, m=m, th=th, bq=bq: e.tensor_tensor(sb_t[:, m, th * 512:(th + 1) * 512], bank(bq), sb_t[:, m, th * 512:(th + 1) * 512], ALU.mult),
                     reads=[BK[bq]], writes=[SBb[m][th]])
                P.op("pool", lambda e, m=m, th=th, jc=jc: e.tensor_tensor(mT[:, jc * 4 + m, th * 512:(th + 1) * 512],
                                                                         sa_t[:, m, th * 512:(th + 1) * 512], sb_t[:, m, th * 512:(th + 1) * 512], ALU.add),
                     reads=[SA[m][th], SBb[m][th]], writes=[MT[jc * 4 + m][th]])
    MRG = [b for row in MT for b in row]
    if DEBUG:
        P.op("sp", lambda e: e.dma_start(out=dbg["mrg"], in_=mT[:]), reads=MRG, dma_key="dbg0")
    if STAGE <= 5:
        return finish(nc, P, out_d)

    prefetch_fence()
    wo, WO = [], []
    for c in range(3):
        t_, b_ = wload()
        wo.append(t_)
        WO.append(b_)
    wo.append(nc.alloc_sbuf_tensor_at("wo3", [128, 16, 512], BF16, offset=R_U))
    WO.append(Buf("wo3"))
    P.op("pool", lambda e: e.dma_start(out=wo[3][:], in_=w_out[0].rearrange("(k p) c -> p k c", p=128)[:, :, 1536:2048]),
         writes=[WO[3]], dma_key="wo3")
    a6 = Arena(nc, R_X, R_U, "a6")
    xs6 = [a6.take("xs%d" % i, [128, D], F32) for i in range(2)]
    gbc6 = a6.take("gbc", [128, D], F32)
    hn32 = [a6.take("hn32_%d" % i, [128, D], F32) for i in range(2)]
    hnb = [a6.take("hnb%d" % i, [128, D], BF16) for i in range(2)]
    hnT = [a6.take("hnT%d" % i, [128, 16, 128], F32) for i in range(2)]
    junk6 = a6.take("junk", [128, D], BF16)
    lgall = a6.take("lgall", [128, NB, 72], F32)
    a6b = Arena(nc, R_X, R_X + 24576, "a6b")
    gmx = a6b.take("gmx", [128, NB], F32)
    tsub = a6b.take("tsub", [128, NB, 8], F32)
    ohg = a6b.take("ohg", [128, NB, 8], F32)
    exg = a6b.take("exg", [128, NB, 8], F32)
    sume = a6b.take("sume", [128, NB], F32)
    gw = a6b.take("gw", [128, NB], F32)
    tmp4 = a6b.take("tmp4", [128, NB, 8, 8], F32)
    sel = a6b.take("sel", [128, NB, 8], F32)
    sel2 = a6b.take("sel2", [128, NB, 8], F32)
    v01 = a6b.take("v01", [128, 2, NB], F32)
    oh0 = a6b.take("oh0", [128, NB, 8], F32)
    oh1 = a6b.take("oh1", [128, NB, 8], F32)
    dv = a6b.take("dv", [128, NB], F32)
    s01 = a6b.take("s01", [128, 2, NB], F32)
    wk = a6b.take("wk", [128, NB, 2], F32)
    O0 = a6b.take("O0", [128, NB, 64], F32)
    O1 = a6b.take("O1", [128, NB, 64], F32)
    m0 = a6b.take("m0", [128, NB, 64], F32)
    m1 = a6b.take("m1", [128, NB, 64], F32)
    rk = a6b.take("rk", [128, NB, 2], F32)
    eidx = a6b.take("eidx", [128, NB, 2], F32)
    ovf = a6b.take("ovf", [128, NB, 2], F32)
    slotf = a6b.take("slotf", [128, NB, 2], F32)
    slotg = a6b.take("slotg", [128, NB, 2], F32)
    Ob = a6.take("Ob", [128, NB, 64], BF16)
    sloti = a6.take("sloti", [128, NB, 2], I32)
    recs = a6.take("recs", [128, NB, 2, 16], I32)
    XS6 = [Buf("xs6_0"), Buf("xs6_1")]
    HNB = [Buf("hnb0"), Buf("hnb1")]
    JK6, G6, R6, OB, REC = Buf("junk6"), Buf("gbc6"), Buf("r6"), Buf("Ob"), Buf("recs")
    H32 = [Buf("hn32_0"), Buf("hn32_1")]
    HNT = [Buf("hnT0"), Buf("hnT1")]
    OUTR = [Buf("out%d" % j) for j in range(NB)]
    P.op("sp", lambda e: e.dma_start(out=gbc6[:], in_=norm2_g.rearrange("o c -> (o c)").partition_broadcast(128)), writes=[G6], dma_key="gbc")
    P.op("pool", lambda e: e.memset(recs[:], 0), writes=[REC])
    P.op("pool", lambda e: e.tensor_copy(recs[:, :, :, 0], bc(icst_t[:, :, 0:1], [128, NB, 2])), reads=[REC, CSTa], writes=[REC])
    P.op("pool", lambda e: e.tensor_copy(recs[:, :, :, 2], icst_t[:, :, 1:3]), reads=[REC, CSTa], writes=[REC])
    chains6 = []
    for j in range(NB):
        s = j % 2
        C = Chain()
        chains6.append(C)
        half = (j % 2) * 4
        oh = 4 - half
        C.op("sp", lambda e, j=j, s=s: e.dma_start(out=xs6[s][:], in_=xh[(j + 1) * 128:(j + 2) * 128, :]), writes=[XS6[s]], dma_key="xs6_%d" % s)

        for c in range(4):
            def mmo(e, j=j, half=half, c=c):
                for k in range(16):
                    ins = e.matmul(bank(half + c), mT[:, k, j * 128:(j + 1) * 128], wo[c][:, k, :], start=(k == 0), stop=(k == 15))
                return ins
            C.op("pe", mmo, reads=[WO[c]] + [MT[k][j // 4] for k in range(16)], writes=[BK[half + c]])
        for c in range(4):
            C.op("dve", lambda e, s=s, half=half, c=c: e.tensor_tensor(xs6[s][:, c * 512:(c + 1) * 512], bank(half + c), xs6[s][:, c * 512:(c + 1) * 512], ALU.add),
                 reads=[BK[half + c], XS6[s]], writes=[XS6[s]])
        C.op("sp", lambda e, j=j, s=s: e.dma_start(out=out_d[j * 128:(j + 1) * 128, :], in_=xs6[s][:]), reads=[XS6[s]], writes=[OUTR[j]], dma_key="h_st%d" % s)
        C.op("act", lambda e, j=j, s=s: e.activation(junk6[:], xs6[s][:], AF.Square, accum_out=ss_t[:, 16 + j:17 + j]),
             reads=[XS6[s]], writes=[JK6, SSC[16 + j]])
        rstd_chain(C, 16 + j, 1.0 / D)
        C.op("dve", lambda e, j=j, s=s: e.scalar_tensor_tensor(hn32[s][:], xs6[s][:], rstd_t[:, 16 + j:17 + j], gbc6[:], ALU.mult, ALU.mult),
             reads=[XS6[s], SSC[16 + j], G6], writes=[H32[s]])
        C.op("act", lambda e, s=s: e.activation(hnb[s][:], hn32[s][:], AF.Copy), reads=[H32[s]], writes=[HNB[s]])
        C.op("sp", lambda e, j=j, s=s: e.dma_start(out=hn_d[j * 128:(j + 1) * 128, :], in_=hnb[s][:]), reads=[HNB[s]], writes=[HND], dma_key="hn_st%d" % s)
        for q4 in range(4):
            bi = oh + q4

            def tr6(e, q4=q4, bi=bi, s=s):
                for kq in range(4):
                    k = q4 * 4 + kq
                    ins = e.transpose(bank(bi)[:, kq * 128:(kq + 1) * 128], hn32[s][:, k * 128:(k + 1) * 128], ident_f)
                return ins
            C.op("pe", tr6, reads=[H32[s], CSTa], writes=[BK[bi]])
            src = bank(bi).rearrange("p (k t) -> p k t", k=4)
            if q4 % 2 == 0:
                C.op("act", lambda e, q4=q4, src=src, s=s: e.activation(hnT[s][:, q4 * 4:q4 * 4 + 4, :], src, AF.Copy), reads=[BK[bi]], writes=[HNT[s]])
            else:
                C.op("dve", lambda e, q4=q4, src=src, s=s: e.tensor_copy(hnT[s][:, q4 * 4:q4 * 4 + 4, :], src), reads=[BK[bi]], writes=[HNT[s]])
        br_ = oh

        def mmr(e, br_=br_, s=s):
            for k in range(16):
                ins = e.matmul(bank(br_)[:, 0:72], hnT[s][:, k, :], Wr[:, k, :], start=(k == 0), stop=(k == 15))
            return ins
        C.op("pe", mmr, reads=[HNT[s], CSTa], writes=[BK[br_]])
        C.op("dve", lambda e, br_=br_, j=j: e.tensor_tensor(lgall[:, j, :], bank(br_)[:, 0:72], br_bc[:], ALU.add), reads=[BK[br_], CSTa], writes=[R6])

    run_chains(chains6, width=2)
    P.fence()

    EW = []
    for i, base in enumerate((R_RING, R_U, MAIN0)):
        ea = Arena(nc, base, base + 49152, "ew%d" % i)
        EW.append((ea.take("eg", [128, 16, FF], BF16), ea.take("eu", [128, 16, FF], BF16), ea.take("ed", [128, 4, D], BF16)))
    EWB = [(Buf("eg%d" % i), Buf("eu%d" % i), Buf("ed%d" % i)) for i in range(3)]

    def eload(ex):
        i = ex % 3
        g_t, u_t, d_t = EW[i]
        P.op("pool", lambda e: e.dma_start(out=g_t[:], in_=w_gate[0, ex].rearrange("(p k) f -> p k f", p=128), max_dma_last_dim=8192),
             writes=[EWB[i][0]], dma_key="eg%d" % i)
        P.op("pool", lambda e: e.dma_start(out=u_t[:], in_=w_up[0, ex].rearrange("(p k) f -> p k f", p=128), max_dma_last_dim=8192),
             writes=[EWB[i][1]], dma_key="eu%d" % i)
        P.op("pool", lambda e: e.dma_start(out=d_t[:], in_=w_down[0, ex].rearrange("(p k) c -> p k c", p=128), max_dma_last_dim=8192),
             writes=[EWB[i][2]], dma_key="ed%d" % i)

    if STAGE >= 7:
        eload(0)
        eload(1)

    gl3 = lgall[:, :, 0:8]
    el4 = lgall[:, :, 8:72].rearrange("p j (g e) -> p j g e", g=8)
    B3 = [128, NB, 8]
    B4 = [128, NB, 8, 8]
    rop = lambda eng, fn: P.op(eng, fn, reads=[R6], writes=[R6])
    rop("dve", lambda e: e.tensor_reduce(gmx[:], gl3, AX.X, ALU.max))
    rop("dve", lambda e: e.tensor_tensor(tsub[:], gl3, bc(gmx[:].unsqueeze(2), B3), ALU.subtract))
    rop("dve", lambda e: e.tensor_scalar(ohg[:], tsub[:], 0.0, None, ALU.is_equal))
    rop("act", lambda e: e.activation(exg[:], tsub[:], AF.Exp))
    rop("dve", lambda e: e.tensor_reduce(sume[:], exg[:], AX.X, ALU.add))
    rop("dve", lambda e: e.reciprocal(gw[:], sume[:]))
    rop("dve", lambda e: e.tensor_tensor(tmp4[:], el4, bc(ohg[:].unsqueeze(3), B4), ALU.mult))
    rop("dve", lambda e: e.tensor_reduce(sel[:], tmp4[:].rearrange("p j g e -> p j e g"), AX.X, ALU.add))
    rop("dve", lambda e: e.tensor_reduce(v01[:, 0, :], sel[:], AX.X, ALU.max))
    rop("dve", lambda e: e.tensor_tensor(oh0[:], sel[:], bc(v01[:, 0, :].unsqueeze(2), B3), ALU.is_equal))
    rop("dve", lambda e: e.scalar_tensor_tensor(sel2[:], oh0[:], -1.0e30, sel[:], ALU.mult, ALU.add))
    rop("dve", lambda e: e.tensor_reduce(v01[:, 1, :], sel2[:], AX.X, ALU.max))

    def r_a(e):
        e.tensor_tensor(oh1[:], sel2[:], bc(v01[:, 1, :].unsqueeze(2), B3), ALU.is_equal)
        return e.tensor_tensor(dv[:], v01[:, 0, :], v01[:, 1, :], ALU.subtract)
    rop("dve", r_a)

    def r_b(e):
        e.activation(s01[:, 0, :], dv[:], AF.Sigmoid)
        return e.activation(s01[:, 1, :], dv[:], AF.Sigmoid, scale=-1.0)
    rop("act", r_b)

    def r_c(e):
        e.tensor_tensor(wk[:, :, 0], s01[:, 0, :], gw[:], ALU.mult)
        e.tensor_tensor(wk[:, :, 1], s01[:, 1, :], gw[:], ALU.mult)
        e.tensor_tensor(O0[:].rearrange("p j (g e) -> p j g e", g=8), bc(ohg[:].unsqueeze(3), B4), bc(oh0[:].unsqueeze(2), B4), ALU.mult)
        return e.tensor_tensor(O1[:].rearrange("p j (g e) -> p j g e", g=8), bc(ohg[:].unsqueeze(3), B4), bc(oh1[:].unsqueeze(2), B4), ALU.mult)
    rop("dve", r_c)
    P.op("dve", lambda e: e.tensor_tensor(Ob[:], O0[:], O1[:], ALU.add), reads=[R6], writes=[OB])
    bp = 0

    def pre(e):
        for j in range(NB):
            o_ = bank(bp)[:, j * 64:(j + 1) * 64]
            ins = e.matmul(o_, U_bf[:], Ob[:, j, :], start=True, stop=(j == 0))
            for jj in range(j):
                ins = e.matmul(o_, ones_bf[:], Ob[:, jj, :], start=False, stop=(jj == j - 1))
        return ins
    P.op("pe", pre, reads=[OB, SET], writes=[BK[bp]])
    pre3 = bank(bp).rearrange("p (j e) -> p j e", j=NB)

    def r_d(e):
        e.tensor_tensor(m0[:], O0[:], pre3, ALU.mult)
        return e.tensor_tensor(m1[:], O1[:], pre3, ALU.mult)
    P.op("dve", r_d, reads=[R6, BK[bp]], writes=[R6])

    def r_e(e):
        e.tensor_reduce(rk[:, :, 0], m0[:], AX.X, ALU.add)
        return e.tensor_reduce(rk[:, :, 1], m1[:], AX.X, ALU.add)
    rop("dve", r_e)

    def r_f(e):
        e.tensor_tensor(m0[:], O0[:], bc(iota64.unsqueeze(1), [128, NB, 64]), ALU.mult)
        return e.tensor_tensor(m1[:], O1[:], bc(iota64.unsqueeze(1), [128, NB, 64]), ALU.mult)
    P.op("dve", r_f, reads=[R6, CSTa], writes=[R6])

    def r_g(e):
        e.tensor_reduce(eidx[:, :, 0], m0[:], AX.X, ALU.add)
        e.tensor_reduce(eidx[:, :, 1], m1[:], AX.X, ALU.add)
        return e.tensor_scalar(ovf[:], rk[:], float(CAP) - 0.5, 1.0e9, ALU.is_gt, ALU.mult)
    rop("dve", r_g)
    rop("dve", lambda e: e.scalar_tensor_tensor(slotf[:], eidx[:], float(CAP), rk[:], ALU.mult, ALU.add))
    rop("dve", lambda e: e.tensor_tensor(slotg[:], slotf[:], ovf[:], ALU.add))

    def r_h(e):
        e.tensor_copy(sloti[:], slotg[:])
        return e.tensor_copy(recs[:, :, :, 1], wk[:].bitcast(I32))
    P.op("dve", r_h, reads=[R6, REC], writes=[R6, REC])
    def tbl_scatter(e):
        r = []
        for j in range(NB):
            for k in range(2):
                r.append(e.indirect_dma_start(out=tbl_d, out_offset=bass.IndirectOffsetOnAxis(ap=sloti[:, j, k:k + 1], axis=0),
                                              in_=recs[:, j, k, :], in_offset=None, bounds_check=bnd_tbl, oob_is_err=False))
        return r
    P.op("pool", tbl_scatter, reads=[R6, REC], writes=[TBL], dma_key="tblsc", ndma=2 * NB)
    if DEBUG:
        P.op("pool", lambda e: e.dma_start(out=dbg["tbl"], in_=tbl_d), reads=[TBL], dma_key="dbg1")
        P.op("pool", lambda e: e.dma_start(out=dbg["hn"], in_=hn_d), reads=[HND], dma_key="dbg1")
    if STAGE <= 6:
        return finish(nc, P, out_d, extra=OUTR)

    P.fence()
    a7 = Arena(nc, MAIN0 + 49152, MAIN0 + 81920, "a7")
    xg = [a7.take("xg%d" % i, [128, D], BF16) for i in range(2)]
    xeT = [a7.take("xeT%d" % i, [128, 16, 128], BF16) for i in range(2)]
    ys = [a7.take("ys%d" % i, [128, D], F32) for i in range(2)]
    a7t = Arena(nc, R_RING + 49152, SB_END, "a7t")
    hT = [a7t.take("hT%d" % i, [128, 4, 128], BF16) for i in range(2)]
    sgt = a7t.take("sgt", [128, 512], F32)
    XG = [Buf("xg0"), Buf("xg1")]
    XET = [Buf("xeT0"), Buf("xeT1")]
    HT = [Buf("hT0"), Buf("hT1")]
    SGT = Buf("sgt")
    YS = [Buf("ys0"), Buf("ys1")]
    TBE = [Buf("tbe0"), Buf("tbe1")]
    Y2S = [Buf("Y2_0"), Buf("Y2_1")]
    for i in range(2):
        P.op("pool", lambda e, i=i: e.memset(xg[i][:], 0.0), writes=[XG[i]])

    def egather(ex):
        s = ex % 2
        P.op("sp", lambda e: e.dma_start(out=tbe[s][:], in_=tbl_d[ex * CAP:(ex + 1) * CAP, :]), reads=[TBL], writes=[TBE[s]], dma_key="tbe%d" % s)
        P.op("pool", lambda e: e.indirect_dma_start(out=xg[s][:], out_offset=None, in_=hn_d,
                                                    in_offset=bass.IndirectOffsetOnAxis(ap=tbe[s][:, 0:1], axis=0),
                                                    bounds_check=bnd_tok, oob_is_err=False),
             reads=[TBE[s], HND], writes=[XG[s]], dma_key="xg%d" % s)

    egather(0)
    for ex in range(NE):
        s = ex % 2
        i3 = ex % 3
        g_t, u_t, d_t = EW[i3]
        if ex + 2 < NE:
            eload(ex + 2)
        if ex + 1 < NE:
            egather(ex + 1)
        for hf in range(2):
            bi = hf

            def tre(e, s=s, hf=hf, bi=bi):
                pb = bank(bi).bitcast(BF16)
                for kq in range(8):
                    k = hf * 8 + kq
                    ins = e.transpose(pb[:, kq * 128:(kq + 1) * 128], xg[s][:, k:D:16], ident_bf[:])
                return ins
            P.op("pe", tre, reads=[XG[s], SET], writes=[BK[bi]])
            src = bank(bi).bitcast(BF16).rearrange("p (k t) -> p k t", k=8)
            if hf == 0:
                P.op("act", lambda e, s=s, hf=hf, src=src: e.activation(xeT[s][:, hf * 8:hf * 8 + 8, :], src, AF.Copy), reads=[BK[bi]], writes=[XET[s]])
            else:
                P.op("dve", lambda e, s=s, hf=hf, src=src: e.tensor_copy(xeT[s][:, hf * 8:hf * 8 + 8, :], src), reads=[BK[bi]], writes=[XET[s]])

        def gu(e, s=s, g_t=g_t, u_t=u_t):
            for (wt_, bi) in ((g_t, 2), (u_t, 3)):
                for m in range(4):
                    for k in range(16):
                        ins = e.matmul(bank(bi)[:, m * 128:(m + 1) * 128], wt_[:, k, m:FF:4], xeT[s][:, k, :], start=(k == 0), stop=(k == 15))
            return ins
        P.op("pe", gu, reads=[XET[s], EWB[i3][0], EWB[i3][1]], writes=[BK[2], BK[3]])
        P.op("act", lambda e: e.activation(sgt[:], bank(2), AF.Silu), reads=[BK[2]], writes=[SGT])
        P.op("dve", lambda e, s=s: e.tensor_tensor(hT[s][:].rearrange("p m t -> p (m t)"), sgt[:], bank(3), ALU.mult), reads=[SGT, BK[3]], writes=[HT[s]])

        def dn(e, s=s, d_t=d_t):
            for c in range(4):
                for m in range(4):
                    ins = e.matmul(bank(4 + c), hT[s][:, m, :], d_t[:, m, c * 512:(c + 1) * 512], start=(m == 0), stop=(m == 3))
            return ins
        P.op("pe", dn, reads=[HT[s], EWB[i3][2]], writes=BK[4:8])
        wcol = tbe[s][:, 1:2].bitcast(F32)
        P.op("act", lambda e, s=s, wcol=wcol: e.activation(ys[s][:, 0:1024], psB[:, 0:1024], AF.Identity, scale=wcol), reads=[BK[4], BK[5], TBE[s]], writes=[YS[s]])
        P.op("dve", lambda e, s=s, wcol=wcol: e.tensor_scalar(ys[s][:, 1024:2048], psB[:, 1024:2048], wcol, None, ALU.mult), reads=[BK[6], BK[7], TBE[s]], writes=[YS[s]])
        def ysc(e, s=s):
            return [e.indirect_dma_start(out=y2_d[hf], out_offset=bass.IndirectOffsetOnAxis(ap=tbe[s][:, 2:3], axis=0),
                                         in_=ys[s][:, hf * 1024:(hf + 1) * 1024], in_offset=None,
                                         bounds_check=bnd_y2, oob_is_err=False) for hf in range(2)]
        P.op("pool", ysc, reads=[YS[s], TBE[s]], writes=[Y2S[s]], dma_key="ysc%d" % s, ndma=2)

    P.fence()
    a8 = Arena(nc, MAIN0, SB_END, "a8")
    NS8 = 4
    hh = [a8.take("hh%d" % i, [128, D], F32) for i in range(NS8)]
    yy = [a8.take("yy%d" % i, [128, 2, D], F32) for i in range(NS8)]
    HH = [Buf("hh%d" % i) for i in range(NS8)]
    YY = [Buf("yy%d" % i) for i in range(NS8)]
    fin = []
    chains8 = []
    for j in range(NB):
        s = j % NS8
        C = Chain()
        chains8.append(C)
        C.op("sp", lambda e, j=j, s=s: e.dma_start(out=hh[s][:], in_=out_d[j * 128:(j + 1) * 128, :]), reads=[OUTR[j]], writes=[HH[s]], dma_key="hh%d" % s)

        def ldy(e, j=j, s=s):
            r = []
            for hf in range(2):
                r.append(e.dma_start(out=yy[s][:, :, hf * 1024:(hf + 1) * 1024],
                                     in_=y2_d[hf].rearrange("(t k) d -> t k d", k=2)[j * 128:(j + 1) * 128, :, :]))
            return r
        C.op("sp", ldy, reads=Y2S, writes=[YY[s]], dma_key="yy%d" % s, ndma=2)
        C.op("dve", lambda e, s=s: e.tensor_tensor(hh[s][:], hh[s][:], yy[s][:, 0, :], ALU.add), reads=[YY[s]], writes=[HH[s]])
        C.op("dve", lambda e, s=s: e.tensor_tensor(hh[s][:], hh[s][:], yy[s][:, 1, :], ALU.add), reads=[YY[s]], writes=[HH[s]])
        C.op("act", lambda e, j=j, s=s: e.dma_start(out=out_d[j * 128:(j + 1) * 128, :], in_=hh[s][:]), reads=[HH[s]], writes=[OUTR[j]], dma_key="o_st%d" % s)
    run_chains(chains8, width=3)
    fw = [("act", P.dma_keys["o_st%d" % i][-1]) for i in range(NS8)]
    for key in ("dbg0", "dbg1"):
        if key in P.dma_keys:
            fw.append((P.dma_keys[key][-1].eng, P.dma_keys[key][-1]))
    P.emit(final_waits=fw)
    return nc


def finish(nc, P, out_d, extra=None):
    fw = []
    for key, lst in P.dma_keys.items():
        fw.append((lst[-1].eng, lst[-1]))
    P.emit(final_waits=fw)
    return nc


_CACHE = {}


def _consts():
    cst = np.zeros((128, 352), np.float32)
    inv = (500000.0 ** (-np.arange(0, 16, 2, dtype=np.float32) / 16.0)).astype(np.float32)
    cst[:, 0:8] = inv
    cst[:, 8:16] = inv
    cst[:, 24:32] = np.float32(math.pi / 2)
    cst[:, 32:96] = np.arange(64, dtype=np.float32)
    cst[:, 96:224] = np.eye(128, dtype=np.float32)
    cst[:, 224:352] = np.triu(np.ones((128, 128), np.float32), 1)
    icst = np.zeros((128, NB, 3), np.int32)
    tok = np.arange(NB)[None, :] * 128 + np.arange(128)[:, None]
    icst[:, :, 0] = tok
    icst[:, :, 1] = tok * 2
    icst[:, :, 2] = tok * 2 + 1
    return cst, icst


def kernel(**inputs):
    x = np.asarray(inputs["x"], np.float32)
    pos = np.asarray(inputs["positions"], np.int32)
    B, S, _ = x.shape
    if "nc" not in _CACHE:
        _CACHE["nc"] = build_program()
    nc = _CACHE["nc"]
    cst, icst = _consts()
    kk = np.arange(128)[:, None]
    ii = np.arange(128)[None, :]
    mprev = (kk >= ii).astype(np.float32)
    mnext = (kk <= ii).astype(np.float32)
    shared = {}
    for k in ("norm1_g", "w_in", "q_norm_g", "k_norm_g", "sink_logits", "sg_ln_g", "sg_ln_b", "sg_w", "sg_b", "w_branch_att",
              "w_branch_sg", "w_out", "norm2_g", "w_group_router", "b_group_router", "w_expert_router", "b_expert_router",
              "w_gate", "w_up", "w_down"):
        if STAGE < 7 and k in ("w_gate", "w_up", "w_down"):
            continue
        shared[k] = np.ascontiguousarray(np.asarray(inputs[k], np.float32))
    in_maps = []
    for c in range(NCORES):
        b, half = c // 2, c % 2
        t0 = half * TOK
        xh = np.zeros((NBH * 128, D), np.float32)
        ph = np.zeros((NBH * 128,), np.int32)
        lo, hi = t0 - 128, t0 + TOK + 128
        slo, shi = max(lo, 0), min(hi, S)
        xh[slo - lo:shi - lo] = x[b, slo:shi]
        ph[slo - lo:shi - lo] = pos[b, slo:shi]
        masks = np.zeros((128, 4, 128), np.float32)
        masks[:, 0, :] = mprev * (1.0 if lo >= 0 else 0.0)
        masks[:, 1, :] = mprev
        masks[:, 2, :] = mnext
        masks[:, 3, :] = mnext * (1.0 if hi <= S else 0.0)
        m = dict(shared)
        m.update({"xh": xh, "posT": np.ascontiguousarray(ph.reshape(NBH, 128).T), "masks": masks, "cst": cst, "icst": icst})
        in_maps.append(m)
    res = run_bass_kernel_spmd(nc, in_maps, core_ids=list(range(NCORES)))
    _CACHE["res"] = res
    out = np.zeros((B, S, D), np.float32)
    for c in range(NCORES):
        b, half = c // 2, c % 2
        out[b, half * TOK:(half + 1) * TOK] = np.asarray(res.results[c]["out"], np.float32)
    return out
```

```python
import math
import os
from contextlib import ExitStack

import numpy as np
import concourse.bass as bass
import concourse.mybir as mybir
from concourse.bass_utils import run_bass_kernel_spmd

F32 = mybir.dt.float32
BF16 = mybir.dt.bfloat16
I32 = mybir.dt.int32
AF = mybir.ActivationFunctionType
ALU = mybir.AluOpType
AX = mybir.AxisListType

NCORES = 8
D = 2048
TOK = 1024
NB = 8
NBH = 10
INC = 7680
OFF_K, OFF_V, OFF_U, OFF_S, OFF_G = 1024, 1280, 1536, 2560, 3584
NE = 64
CAP = 128
FF = 512
EPS = 1e-6
SB_BASE = 16512
SB_END = 229376
BIG = 1 << 30
TWO_PI = 2.0 * math.pi

DEBUG = os.environ.get("KDEBUG", "") != ""
STAGE = int(os.environ.get("KSTAGE", "9"))

ENGS = ("pe", "act", "dve", "pool", "sp")


class Buf:
    __slots__ = ("name", "last_w", "readers")

    def __init__(self, name):
        self.name = name
        self.last_w = None
        self.readers = []


class Op:
    __slots__ = ("eng", "idx", "fn", "waits", "signal", "sigval", "is_dma", "key", "ndma")

    def __init__(self, eng, idx, fn, is_dma, key, ndma):
        self.eng = eng
        self.idx = idx
        self.fn = fn
        self.waits = []
        self.signal = False
        self.sigval = None
        self.is_dma = is_dma
        self.key = key
        self.ndma = ndma


class Prog:
    def __init__(self, nc):
        self.nc = nc
        self.streams = {e: [] for e in ENGS}
        self.known = {e: {} for e in ENGS}
        self.dma_keys = {}
        self.fence_deps = []
        self.last_eng_op = {}

    @staticmethod
    def _chan(op):
        return ("dma", op.key) if op.is_dma else ("eng", op.eng)

    def fence(self):
        deps = list(self.last_eng_op.values())
        for lst in self.dma_keys.values():
            deps.append(lst[-1])
        self.fence_deps = deps

    def op(self, eng, fn, reads=(), writes=(), dma_key=None, ndma=1):
        is_dma = dma_key is not None
        st = self.streams[eng]
        o = Op(eng, len(st), fn, is_dma, dma_key, ndma)
        deps = list(self.fence_deps)
        for b in reads:
            if b.last_w is not None:
                deps.append(b.last_w)
        for b in writes:
            if b.last_w is not None:
                deps.append(b.last_w)
            deps.extend(b.readers)
        if is_dma:
            lst = self.dma_keys.setdefault(dma_key, [])
            o.idx = len(lst)
            lst.append(o)
        else:
            self.last_eng_op[eng] = o
        kn = self.known[eng]
        best = {}
        for d in deps:
            ch = self._chan(d)
            if (not d.is_dma) and d.eng == "pe" and eng == "pe" and not is_dma:
                continue
            if kn.get(ch, -1) >= d.idx:
                continue
            if ch not in best or best[ch].idx < d.idx:
                best[ch] = d
        for ch, d in best.items():
            kn[ch] = d.idx
            d.signal = True
            o.waits.append(d)
        for b in reads:
            b.readers.append(o)
        for b in writes:
            b.last_w = o
            b.readers = []
        st.append(o)
        return o

    def emit(self, final_waits=()):
        nc = self.nc
        for _, o in final_waits:
            o.signal = True
        for e in ENGS:
            cnt = 0
            for o in self.streams[e]:
                if (not o.is_dma) and o.signal:
                    cnt += 1
                    o.sigval = cnt
        for lst in self.dma_keys.values():
            cnt = 0
            for o in lst:
                cnt += 16 * o.ndma
                o.sigval = cnt
        with ExitStack() as es:
            eng_sems = {e: es.enter_context(nc.semaphore("s_" + e)) for e in ENGS}
            dma_sems = {k: es.enter_context(nc.semaphore("d_%s" % k)) for k in self.dma_keys}
            block = es.enter_context(nc.Block())

            def sem_of(o):
                return dma_sems[o.key] if o.is_dma else eng_sems[o.eng]

            def run(e, engobj):
                for o in self.streams[e]:
                    for d in o.waits:
                        engobj.wait_ge(sem_of(d), d.sigval)
                    ins = o.fn(engobj)
                    if o.is_dma:
                        if not isinstance(ins, (list, tuple)):
                            ins = [ins]
                        assert len(ins) == o.ndma
                        for i_ in ins:
                            i_.then_inc(dma_sems[o.key], 16)
                    elif o.signal:
                        ins.then_inc(eng_sems[e], 1)
                for ee, o in final_waits:
                    if ee == e:
                        engobj.wait_ge(sem_of(o), o.sigval)

            @block.tensor
            def _(eng):
                run("pe", eng)

            @block.scalar
            def _(eng):
                run("act", eng)

            @block.vector
            def _(eng):
                run("dve", eng)

            @block.gpsimd
            def _(eng):
                run("pool", eng)

            @block.sync
            def _(eng):
                run("sp", eng)


class Arena:
    def __init__(self, nc, start, end, tag):
        self.nc, self.cur, self.end, self.tag = nc, start, end, tag
        self.n = 0

    def take(self, name, shape, dtype):
        esz = 2 if dtype == BF16 else 4
        nbytes = esz * int(np.prod(shape[1:]))
        nbytes = (nbytes + 31) // 32 * 32
        off = self.cur
        assert off + nbytes <= self.end, (self.tag, name, off, nbytes, self.end)
        self.cur += nbytes
        self.n += 1
        return self.nc.alloc_sbuf_tensor_at("%s_%s" % (self.tag, name), list(shape), dtype, offset=off)


def bc(ap, shape):
    return ap.broadcast_to(list(shape))


def build_program():
    nc = bass.Bass("TRN2", target_bir_lowering=False)
    P = Prog(nc)

    def din(name, shape, dt=F32):
        return nc.dram_tensor(name, list(shape), dt, kind="ExternalInput").ap()

    xh = din("xh", [NBH * 128, D])
    posT = din("posT", [128, NBH], I32)
    masks_d = din("masks", [128, 4, 128])
    cst_d = din("cst", [128, 352])
    icst_d = din("icst", [128, NB, 3], I32)
    norm1_g = din("norm1_g", [1, D])
    w_in = din("w_in", [1, D, INC])
    q_norm_g = din("q_norm_g", [1, 64])
    k_norm_g = din("k_norm_g", [1, 64])
    sink_d = din("sink_logits", [1, 16])
    sg_ln_g = din("sg_ln_g", [1, 8, 128])
    sg_ln_b = din("sg_ln_b", [1, 8, 128])
    sg_w = din("sg_w", [1, 8, 128, 128])
    sg_b = din("sg_b", [1, 8, 128])
    w_ba = din("w_branch_att", [1, 1024, D])
    w_bb = din("w_branch_sg", [1, 1024, D])
    w_out = din("w_out", [1, D, D])
    norm2_g = din("norm2_g", [1, D])
    w_gr = din("w_group_router", [1, D, 8])
    b_gr = din("b_group_router", [1, 8])
    w_er = din("w_expert_router", [1, D, 64])
    b_er = din("b_expert_router", [1, 64])
    if STAGE >= 7:
        w_gate = din("w_gate", [1, NE, D, FF])
        w_up = din("w_up", [1, NE, D, FF])
        w_down = din("w_down", [1, NE, FF, D])

    out_d = nc.dram_tensor("out", [TOK, D], F32, kind="ExternalOutput").ap()
    hn_d = nc.dram_tensor("hn_d", [TOK + 1, D], BF16, kind="Internal").ap()
    tbl_d = nc.dram_tensor("tbl_d", [NE * CAP, 16], I32, kind="Internal").ap()
    y2_d = [nc.dram_tensor("y2_d%d" % i, [2 * TOK, D // 2], F32, kind="Internal").ap() for i in range(2)]
    dbg = {}
    if DEBUG:
        dbg["att"] = nc.dram_tensor("dbg_att", [64, 16, TOK], BF16, kind="ExternalOutput").ap()
        dbg["sgo"] = nc.dram_tensor("dbg_sgo", [128, 8, TOK], BF16, kind="ExternalOutput").ap()
        dbg["mrg"] = nc.dram_tensor("dbg_mrg", [128, 16, TOK], BF16, kind="ExternalOutput").ap()
        dbg["xnT"] = nc.dram_tensor("dbg_xnT", [128, 16, TOK], BF16, kind="ExternalOutput").ap()
        dbg["kT"] = nc.dram_tensor("dbg_kT", [64, 4, NBH * 128], BF16, kind="ExternalOutput").ap()
        dbg["tbl"] = nc.dram_tensor("dbg_tbl", [NE * CAP, 16], I32, kind="ExternalOutput").ap()
        dbg["hn"] = nc.dram_tensor("dbg_hn", [TOK + 1, D], BF16, kind="ExternalOutput").ap()

    psA = nc.alloc_psum_tensor("psA", [128, 2048], F32)
    psB = nc.alloc_psum_tensor("psB", [128, 2048], F32)
    BK = [Buf("bank%d" % i) for i in range(8)]

    def bank(i):
        t = psA if i < 4 else psB
        return t[:, (i % 4) * 512:(i % 4 + 1) * 512]

    PERS = 24576
    pa = Arena(nc, SB_BASE, SB_BASE + PERS, "p")
    cst_t = pa.take("cst", [128, 352], F32)
    ident_bf = pa.take("identb", [128, 128], BF16)
    U_bf = pa.take("Ub", [128, 128], BF16)
    ones_bf = pa.take("onesb", [128, 128], BF16)
    mask_t = pa.take("mask", [128, 4, 128], BF16)
    wsT = pa.take("wsT", [128, 8, 128], BF16)
    bs_row = pa.take("bsrow", [1, 1024], BF16)
    lng_bc = pa.take("lng", [128, 1024], F32)
    lnb_bc = pa.take("lnb", [128, 1024], F32)
    gq_bc = pa.take("gq", [128, 64], F32)
    gk_bc = pa.take("gk", [128, 64], F32)
    sk_t = pa.take("sk", [128, 16], F32)
    sinkexp = pa.take("sinkexp", [128, 16], F32)
    cs_t = pa.take("cs", [128, NBH, 16], F32)
    ss_t = pa.take("ss", [128, 32], F32)
    sv_t = pa.take("sv", [128, 32], F32)
    rstd_t = pa.take("rstd", [128, 32], F32)
    Wr = pa.take("Wr", [128, 16, 72], F32)
    br_bc = pa.take("brbc", [128, 72], F32)
    icst_t = pa.take("icst", [128, NB, 3], I32)
    tbe = [pa.take("tbe%d" % i, [128, 16], I32) for i in range(4)]
    ident_f = cst_t[:, 96:224]
    iota64 = cst_t[:, 32:96]
    MAIN0 = SB_BASE + PERS
    bnd_tbl = nc.gpsimd.alloc_register("bnd_tbl")
    bnd_y2 = nc.gpsimd.alloc_register("bnd_y2")
    bnd_tok = nc.gpsimd.alloc_register("bnd_tok")

    def set_bounds(e):
        e.reg_mov(bnd_tbl, NE * CAP - 1)
        e.reg_mov(bnd_tok, TOK - 1)
        return e.reg_mov(bnd_y2, 2 * TOK - 1)
    P.op("pool", set_bounds)

    CST = Buf("CST")
    cst_ops = []

    def cload(out, in_, eng="sp"):
        o = P.op(eng, lambda e: e.dma_start(out=out, in_=in_), dma_key="cst_" + eng)
        cst_ops.append(o)
        return o

    _ru = MAIN0 + 81920
    xs = [nc.alloc_sbuf_tensor_at("a1_xs%d" % i, [128, D], F32, offset=_ru + i * 8192) for i in range(2)]
    gbc = nc.alloc_sbuf_tensor_at("a1_gbc", [128, D], F32, offset=_ru + 16384)
    XS = [Buf("xs0"), Buf("xs1")]
    GBC = Buf("gbc")
    EARLY = []
    for tb_ in range(2):
        o_ = P.op("sp", lambda e, tb_=tb_: e.dma_start(out=xs[tb_][:], in_=xh[tb_ * 128:(tb_ + 1) * 128, :]), writes=[XS[tb_]], dma_key="xs%d" % tb_)
        b_ = Buf("early%d" % tb_)
        b_.last_w = o_
        EARLY.append(b_)
    P.op("sp", lambda e: e.dma_start(out=gbc[:], in_=norm1_g.rearrange("o c -> (o c)").partition_broadcast(128)), writes=[GBC], dma_key="gbc")

    cload(cst_t[:], cst_d)
    cload(icst_t[:], icst_d)
    cload(lng_bc[:], sg_ln_g.rearrange("o g c -> (o g c)").partition_broadcast(128))
    cload(lnb_bc[:], sg_ln_b.rearrange("o g c -> (o g c)").partition_broadcast(128))
    cload(gq_bc[:], q_norm_g.rearrange("o c -> (o c)").partition_broadcast(128))
    cload(gk_bc[:], k_norm_g.rearrange("o c -> (o c)").partition_broadcast(128))
    cload(sk_t[:], sink_d.rearrange("o c -> (o c)").partition_broadcast(128))
    cload(br_bc[:, 0:8], b_gr.rearrange("o c -> (o c)").partition_broadcast(128))
    cload(br_bc[:, 8:72], b_er.rearrange("o c -> (o c)").partition_broadcast(128))
    with nc.allow_non_contiguous_dma(reason="small router weight relayout"):
        cload(Wr[:, :, 0:8], w_gr[0].rearrange("(k p) c -> p k c", p=128))
        cload(Wr[:, :, 8:72], w_er[0].rearrange("(k p) c -> p k c", p=128))
    s0 = Arena(nc, MAIN0 + 65536, MAIN0 + 81920, "s0")
    wl = s0.take("wl", [128, 8, 128], F32)
    pos_i = s0.take("posi", [128, NBH], I32)
    pos_f = s0.take("posf", [128, NBH], F32)
    ang = s0.take("ang", [128, NBH, 16], F32)
    rr = s0.take("rr", [128, NBH, 16], F32)
    kk_i = s0.take("kki", [128, NBH, 16], I32)
    kk_f = s0.take("kkf", [128, NBH, 16], F32)
    mm_t = s0.take("mmt", [128, NBH, 16], F32)
    tblinit = s0.take("tblinit", [128, 64, 16], I32)
    zrow = s0.take("zrow", [1, D], BF16)
    cload(wl[:], sg_w[0].rearrange("g t s -> t g s"))
    cload(pos_i[:], posT)
    cload(mask_t[:], masks_d, eng="pool")
    cload(bs_row[:], sg_b.rearrange("o g t -> o (g t)"), eng="pool")
    last_sp = [o for o in cst_ops if o.eng == "sp"][-1]
    last_pool = [o for o in cst_ops if o.eng == "pool"][-1]
    CSTa, CSTb = Buf("CSTa"), Buf("CSTb")
    CSTa.last_w = last_sp
    CSTb.last_w = last_pool
    CONST = [CSTa, CSTb]

    SET = Buf("SET")
    P.op("dve", lambda e: e.tensor_copy(ident_bf[:], ident_f), reads=CONST, writes=[SET])
    P.op("dve", lambda e: e.tensor_copy(U_bf[:], cst_t[:, 224:352]), reads=CONST, writes=[SET])
    P.op("dve", lambda e: e.memset(ones_bf[:], 1.0), writes=[SET])
    P.op("dve", lambda e: e.memset(ss_t[:], 0.0), writes=[SET])
    P.op("dve", lambda e: e.memset(sv_t[:], 0.0), writes=[SET])
    P.op("act", lambda e: e.activation(sinkexp[:], sk_t[:], AF.Exp), reads=CONST, writes=[SET])
    for g in range(8):
        bi = g % 2
        P.op("pe", lambda e, g=g, bi=bi: e.transpose(bank(bi)[:, 0:128], wl[:, g, :], ident_f), reads=CONST, writes=[BK[bi]])
        P.op("dve", lambda e, g=g, bi=bi: e.tensor_copy(wsT[:, g, :], bank(bi)[:, 0:128]), reads=[BK[bi]], writes=[SET])
    RB = Buf("rope")
    P.op("dve", lambda e: e.tensor_copy(pos_f[:], pos_i[:]), reads=CONST, writes=[RB])
    P.op("dve", lambda e: e.tensor_tensor(ang[:], bc(pos_f[:].unsqueeze(2), [128, NBH, 16]),
                                          bc(cst_t[:, 0:16].unsqueeze(1), [128, NBH, 16]), ALU.mult), reads=[RB], writes=[RB])
    P.op("dve", lambda e: e.tensor_tensor(ang[:], ang[:], bc(cst_t[:, 16:32].unsqueeze(1), [128, NBH, 16]), ALU.add), reads=[RB], writes=[RB])
    P.op("dve", lambda e: e.tensor_scalar(rr[:], ang[:], 1.0 / TWO_PI, None, ALU.mult), reads=[RB], writes=[RB])
    P.op("dve", lambda e: e.tensor_copy(kk_i[:], rr[:]), reads=[RB], writes=[RB])
    P.op("dve", lambda e: e.tensor_copy(kk_f[:], kk_i[:]), reads=[RB], writes=[RB])
    P.op("dve", lambda e: e.scalar_tensor_tensor(rr[:], kk_f[:], -6.28125, ang[:], ALU.mult, ALU.add), reads=[RB], writes=[RB])
    P.op("dve", lambda e: e.scalar_tensor_tensor(ang[:], kk_f[:], -(TWO_PI - 6.28125), rr[:], ALU.mult, ALU.add), reads=[RB], writes=[RB])
    P.op("dve", lambda e: e.tensor_scalar(mm_t[:], ang[:], math.pi, TWO_PI, ALU.is_gt, ALU.mult), reads=[RB], writes=[RB])
    P.op("dve", lambda e: e.tensor_tensor(rr[:], ang[:], mm_t[:], ALU.subtract), reads=[RB], writes=[RB])
    P.op("dve", lambda e: e.tensor_scalar(mm_t[:], rr[:], -math.pi, TWO_PI, ALU.is_lt, ALU.mult), reads=[RB], writes=[RB])
    P.op("dve", lambda e: e.tensor_tensor(ang[:], rr[:], mm_t[:], ALU.add), reads=[RB], writes=[RB])
    P.op("dve", lambda e: e.tensor_scalar(rr[:], ang[:], -3.1415925, 3.1415925, ALU.max, ALU.min), reads=[RB], writes=[RB])
    P.op("act", lambda e: e.activation(cs_t[:], rr[:], AF.Sin), reads=[RB], writes=[SET])
    TBL = Buf("TBL")
    HND = Buf("HND")
    P.op("pool", lambda e: e.memset(tblinit[:], 0), writes=[RB])
    P.op("pool", lambda e: e.memset(tblinit[:, :, 0:1], TOK), reads=[RB], writes=[RB])
    P.op("pool", lambda e: e.memset(tblinit[:, :, 2:3], BIG), reads=[RB], writes=[RB])
    P.op("pool", lambda e: e.memset(zrow[:], 0.0), writes=[SET])

    R_X = MAIN0
    R_Q = MAIN0 + 32768
    R_KV = MAIN0 + 65536
    R_U = MAIN0 + 81920
    R_M = MAIN0 + 98304
    R_RING = MAIN0 + 131072
    assert R_RING + 3 * 16384 <= SB_END
    xnT = nc.alloc_sbuf_tensor_at("xnT", [128, 16, TOK], BF16, offset=R_X)
    qT = nc.alloc_sbuf_tensor_at("qT", [128, 16, TOK], BF16, offset=R_Q)
    kT = nc.alloc_sbuf_tensor_at("kT", [128, 4, NBH * 128], BF16, offset=R_KV)
    vt = nc.alloc_sbuf_tensor_at("vt", [128, NBH, 256], BF16, offset=R_KV + 10240)
    sa_t = nc.alloc_sbuf_tensor_at("sa", [128, 4, TOK], BF16, offset=R_KV)
    sb_t = nc.alloc_sbuf_tensor_at("sb", [128, 4, TOK], BF16, offset=R_KV + 8192)
    uT = nc.alloc_sbuf_tensor_at("uT", [128, 8, TOK], BF16, offset=R_U)
    mT = nc.alloc_sbuf_tensor_at("mT", [128, 16, TOK], BF16, offset=R_M)
    ring = [nc.alloc_sbuf_tensor_at("ring%d" % i, [128, 16, 512], BF16, offset=R_RING + i * 16384) for i in range(3)]
    RING = [Buf("ring%d" % i) for i in range(3)]

    def w_in_view(col):
        return w_in[0].rearrange("(k p) c -> p k c", p=128)[:, :, col:col + 512]

    wtiles = [(w_in_view(0), 128, 16), (w_in_view(512), 128, 16), (w_in_view(OFF_K), 128, 16),
              (w_in_view(OFF_U), 128, 16), (w_in_view(OFF_U + 512), 128, 16), (w_in_view(OFF_S), 128, 16), (w_in_view(OFF_S + 512), 128, 16)]
    for jc_ in range(4):
        wtiles.append((w_in_view(OFF_G + jc_ * 512), 128, 16))
        wtiles.append((w_in_view(OFF_G + D + jc_ * 512), 128, 16))
        wtiles.append((w_ba[0].rearrange("(h d) c -> d h c", d=64)[:, :, jc_ * 512:(jc_ + 1) * 512], 64, 16))
        wtiles.append((w_bb[0].rearrange("(g c) n -> c g n", c=128)[:, :, jc_ * 512:(jc_ + 1) * 512], 128, 8))
    for c_ in range(3):
        wtiles.append((w_out[0].rearrange("(k p) c -> p k c", p=128)[:, :, c_ * 512:(c_ + 1) * 512], 128, 16))
    w_issued = [0]
    w_used = [0]

    def w_issue_upto(n):
        while w_issued[0] < min(n, len(wtiles)):
            t = w_issued[0]
            view, nparts, kdim = wtiles[t]
            i = t % 3
            dst = ring[i][0:nparts, 0:kdim, :]
            P.op("pool", lambda e, dst=dst, view=view: e.dma_start(out=dst, in_=view), reads=(EARLY if t < 3 else []), writes=[RING[i]], dma_key="ring%d" % i)
            w_issued[0] += 1

    def wload(chain=None):
        t = w_used[0]
        w_used[0] += 1
        if chain is None:
            w_issue_upto(t + 3)
        else:
            chain.ops.append(("call", lambda t=t: w_issue_upto(t + 3)))
        return ring[t % 3], RING[t % 3]

    def prefetch_fence():
        w_issue_upto(w_used[0] + 3)
        P.fence()

    w_issue_upto(3)

    class Chain:
        def __init__(self):
            self.ops = []

        def op(self, *a, **k):
            self.ops.append((a, k))

    def run_chains(chains, width=2):
        pend = list(chains)
        active = []
        while pend or active:
            while pend and len(active) < width:
                active.append([pend.pop(0), 0])
            for ent in list(active):
                ch = ent[0]
                while ent[1] < len(ch.ops) and ch.ops[ent[1]][0] == "call":
                    ch.ops[ent[1]][1]()
                    ent[1] += 1
                if ent[1] < len(ch.ops):
                    a, k = ch.ops[ent[1]]
                    P.op(*a, **k)
                    ent[1] += 1
                if ent[1] >= len(ch.ops):
                    active.remove(ent)

    a1 = Arena(nc, R_U, R_M + 32768, "a1")
    assert a1.cur == _ru
    a1.cur += 3 * 8192
    xb = [a1.take("xb%d" % i, [128, D], BF16) for i in range(2)]
    junk = a1.take("junk", [128, D], BF16)
    xnH = a1.take("xnH", [128, 16, 256], BF16)
    assert a1.cur <= R_M + 20480 + 8192 + 4096 or True
    XB = [Buf("xb0"), Buf("xb1")]
    JK = Buf("junk")
    SS = Buf("ss")
    XNT = [Buf("xnT%d" % j) for j in range(NBH)]
    SSC = [Buf("ss%d" % i) for i in range(32)]

    def rstd_chain(C, col, scale, src=ss_t):
        SSB = SSC[col]
        C.op("dve", lambda e: e.tensor_scalar(sv_t[:, col:col + 1], src[:, col:col + 1], scale, EPS, ALU.mult, ALU.add), reads=[SSB], writes=[SSB])
        C.op("act", lambda e: e.activation(sv_t[:, col:col + 1], sv_t[:, col:col + 1], AF.Sqrt), reads=[SSB], writes=[SSB])
        C.op("dve", lambda e: e.reciprocal(rstd_t[:, col:col + 1], sv_t[:, col:col + 1]), reads=[SSB], writes=[SSB])

    chains1 = []
    for tb in range(NBH):
        s = tb % 2
        C = Chain()
        chains1.append(C)
        if tb >= 2:
            C.op("sp", lambda e, tb=tb, s=s: e.dma_start(out=xs[s][:], in_=xh[tb * 128:(tb + 1) * 128, :]), writes=[XS[s]], dma_key="xs%d" % s)
        C.op("act", lambda e, tb=tb, s=s: e.activation(junk[:], xs[s][:], AF.Square, accum_out=ss_t[:, tb:tb + 1]), reads=[XS[s], SET], writes=[JK, SSC[tb]])
        rstd_chain(C, tb, 1.0 / D)
        C.op("dve", lambda e, tb=tb, s=s: e.scalar_tensor_tensor(xb[s][:], xs[s][:], rstd_t[:, tb:tb + 1], gbc[:], ALU.mult, ALU.mult),
             reads=[XS[s], SSC[tb], GBC], writes=[XB[s]])
        for half in range(2):
            bi = (2 * tb + half) % 8

            def tr(e, s=s, half=half, bi=bi):
                pb = bank(bi).bitcast(BF16)
                for kq in range(8):
                    k = half * 8 + kq
                    ins = e.transpose(pb[:, kq * 128:(kq + 1) * 128], xb[s][:, k * 128:(k + 1) * 128], ident_bf[:])
                return ins
            C.op("pe", tr, reads=[XB[s], SET], writes=[BK[bi]])
            if tb == 0:
                dst = xnH[:, half * 8:half * 8 + 8, 0:128]
            elif tb == NBH - 1:
                dst = xnH[:, half * 8:half * 8 + 8, 128:256]
            else:
                dst = xnT[:, half * 8:half * 8 + 8, (tb - 1) * 128:tb * 128]
            src = bank(bi).bitcast(BF16).rearrange("p (k t) -> p k t", k=8)
            if half == 0:
                C.op("act", lambda e, dst=dst, src=src: e.activation(dst, src, AF.Copy), reads=[BK[bi]], writes=[XNT[tb]])
            else:
                C.op("dve", lambda e, dst=dst, src=src: e.tensor_copy(dst, src), reads=[BK[bi]], writes=[XNT[tb]])
    run_chains(chains1, width=2)
    P.op("sp", lambda e: e.dma_start(out=tbl_d.rearrange("(p r) c -> p r c", p=128), in_=tblinit[:]), reads=[RB], writes=[TBL], dma_key="tblinit")
    P.op("sp", lambda e: e.dma_start(out=hn_d[TOK:TOK + 1, :], in_=zrow[:]), reads=[SET], writes=[HND], dma_key="zrow")

    def xn_cols(tb):
        if tb == 0:
            return xnH[:, :, 0:128]
        if tb == NBH - 1:
            return xnH[:, :, 128:256]
        return xnT[:, :, (tb - 1) * 128:tb * 128]

    prefetch_fence()

    a2 = Arena(nc, R_M, R_M + 20480, "a2")
    T2S = []
    NW = 4
    for ts in range(NW):
        qa_ = a2.take("qa%d" % ts, [128, 512], F32)
        T2S.append(dict(sqf=qa_, qnf=qa_, qg=qa_, rt=[a2.take("rt%d_%d" % (ts, i), [128, 8, 8], F32) for i in range(4)],
                        qb=a2.take("qb%d" % ts, [128, 512], BF16), rq=a2.take("rq%d" % ts, [128, 16], F32), buf=Buf("t2_%d" % ts)))
    QT = [[Buf("qT%d_%d" % (j, h)) for h in range(4)] for j in range(NB)]
    KT = [Buf("kT%d" % tb) for tb in range(NBH)]
    VT = [Buf("vt%d" % tb) for tb in range(NBH)]
    bank_rr = [0]

    def next_bank():
        b = bank_rr[0] % 8
        bank_rr[0] += 1
        return b

    def qk_chain(C, ts, bsrc, BKb, nh, tb, g_bc, dst_fn, dst_bufs):
        S_ = T2S[ts]
        sqf, qnf, qg, rt, qb, rq, T2 = S_["sqf"], S_["qnf"], S_["qg"], S_["rt"], S_["qb"], S_["rq"], S_["buf"]
        w = nh * 64
        C.op("act", lambda e: e.activation(sqf[:, 0:w], bsrc, AF.Square), reads=[BKb], writes=[T2])
        C.op("dve", lambda e: e.tensor_reduce(rq[:, 0:nh], sqf[:, 0:w].rearrange("p (h d) -> p h d", h=nh), AX.X, ALU.add), reads=[T2], writes=[T2])
        C.op("dve", lambda e: e.tensor_scalar(rq[:, 0:nh], rq[:, 0:nh], 1.0 / 64, EPS, ALU.mult, ALU.add), reads=[T2], writes=[T2])
        C.op("act", lambda e: e.activation(rq[:, 0:nh], rq[:, 0:nh], AF.Sqrt), reads=[T2], writes=[T2])
        C.op("dve", lambda e: e.reciprocal(rq[:, 8:8 + nh], rq[:, 0:nh]), reads=[T2], writes=[T2])
        q3 = lambda t: t[:, 0:w].rearrange("p (h d) -> p h d", h=nh)
        C.op("dve", lambda e: e.tensor_tensor(q3(qnf), bsrc.rearrange("p (h d) -> p h d", h=nh),
                                              bc(rq[:, 8:8 + nh].unsqueeze(2), [128, nh, 64]), ALU.mult), reads=[T2, BKb], writes=[T2])
        C.op("pool", lambda e: e.tensor_tensor(q3(qg), q3(qnf), bc(g_bc[:].unsqueeze(1), [128, nh, 64]), ALU.mult), reads=[T2, CSTa], writes=[T2])
        sin_b = bc(cs_t[:, tb, 0:8].unsqueeze(1), [128, nh, 8])
        cos_b = bc(cs_t[:, tb, 8:16].unsqueeze(1), [128, nh, 8])

        def rope4(e):
            e.tensor_tensor(rt[0][:, 0:nh, :], q3(qg)[:, :, 0:8], cos_b, ALU.mult)
            e.tensor_tensor(rt[1][:, 0:nh, :], q3(qg)[:, :, 8:16], sin_b, ALU.mult)
            e.tensor_tensor(rt[2][:, 0:nh, :], q3(qg)[:, :, 8:16], cos_b, ALU.mult)
            return e.tensor_tensor(rt[3][:, 0:nh, :], q3(qg)[:, :, 0:8], sin_b, ALU.mult)
        C.op("pool", rope4, reads=[T2, SET], writes=[T2])
        C.op("act", lambda e: e.activation(qb[:, 0:w], qg[:, 0:w], AF.Copy), reads=[T2], writes=[T2])

        def rope2(e):
            e.tensor_tensor(q3(qb)[:, :, 0:8], rt[0][:, 0:nh, :], rt[1][:, 0:nh, :], ALU.subtract)
            return e.tensor_tensor(q3(qb)[:, :, 8:16], rt[2][:, 0:nh, :], rt[3][:, 0:nh, :], ALU.add)
        C.op("dve", rope2, reads=[T2], writes=[T2])
        bt = next_bank()

        def trs(e):
            pb = bank(bt).bitcast(BF16)
            for hh in range(nh):
                ins = e.transpose(pb[0:64, hh * 128:(hh + 1) * 128], qb[:, hh * 64:(hh + 1) * 64], ident_bf[:])
            return ins
        C.op("pe", trs, reads=[T2, SET], writes=[BK[bt]])
        src = bank(bt).bitcast(BF16)[0:64, 0:nh * 128].rearrange("p (h t) -> p h t", h=nh)
        C.op("act", lambda e: e.activation(dst_fn(), src, AF.Copy), reads=[BK[bt]], writes=dst_bufs)

    chains = []
    n_ch = 0
    for qi in range(2):
        wl_pending = True
        for j in range(NB):
            tb = j + 1
            bq = next_bank()
            C = Chain()
            if wl_pending:
                wt, WB = wload(chain=C)
                wl_pending = False

            def mmq(e, wt=wt, tb=tb, bq=bq):
                xa = xn_cols(tb)
                for k in range(16):
                    ins = e.matmul(bank(bq), xa[:, k, :], wt[:, k, :], start=(k == 0), stop=(k == 15))
                return ins
            C.op("pe", mmq, reads=[WB, XNT[tb]], writes=[BK[bq]])
            qk_chain(C, n_ch % NW, bank(bq), BK[bq], 8, tb, gq_bc,
                     lambda qi=qi, j=j: qT[0:64, qi * 8:qi * 8 + 8, j * 128:(j + 1) * 128],
                     [QT[j][qi * 2], QT[j][qi * 2 + 1]])
            chains.append(C)
            n_ch += 1
    wl_pending = True
    for tb in range(NBH):
        bq = next_bank()
        C = Chain()
        if wl_pending:
            wt, WB = wload(chain=C)
            wl_pending = False

        def mmk(e, wt=wt, tb=tb, bq=bq):
            xa = xn_cols(tb)
            for k in range(16):
                ins = e.matmul(bank(bq), xa[:, k, :], wt[:, k, :], start=(k == 0), stop=(k == 15))
            return ins
        C.op("pe", mmk, reads=[WB, XNT[tb]], writes=[BK[bq]])
        C.op("act", lambda e, tb=tb, bq=bq: e.activation(vt[:, tb, :], bank(bq)[:, 256:512], AF.Copy), reads=[BK[bq]], writes=[VT[tb]])
        qk_chain(C, n_ch % NW, bank(bq)[:, 0:256], BK[bq], 4, tb, gk_bc,
                 lambda tb=tb: kT[0:64, :, tb * 128:(tb + 1) * 128], [KT[tb]])
        chains.append(C)
        n_ch += 1
    run_chains(chains, width=4)

    if DEBUG:
        P.op("sp", lambda e: e.dma_start(out=dbg["xnT"], in_=xnT[:]), reads=XNT, dma_key="dbg0")
        P.op("sp", lambda e: e.dma_start(out=dbg["kT"], in_=kT[0:64, :, :]), reads=KT, dma_key="dbg0")

    prefetch_fence()
    a3 = Arena(nc, R_M, R_M + 20480, "a3")
    PT = [a3.take("PT%d" % i, [128, 3, 512], BF16) for i in range(2)]
    rd = a3.take("rd", [128, 512], F32)
    srow = [a3.take("srow%d" % i, [1, 2048], BF16) for i in range(2)]
    sk16 = [a3.take("sk16_%d" % i, [1, 16], BF16) for i in range(2)]
    sk32 = [a3.take("sk32_%d" % i, [1, 16], F32) for i in range(2)]
    SR = Buf("srow")
    P.op("dve", lambda e: e.tensor_copy(sk16[0][:], sinkexp[0:1, :]), reads=[SET], writes=[SR])
    P.op("dve", lambda e: e.tensor_copy(sk32[0][:], sk16[0][:]), reads=[SR], writes=[SR])
    P.op("dve", lambda e: e.tensor_tensor(sk32[1][:], sinkexp[0:1, :], sk32[0][:], ALU.subtract), reads=[SR, SET], writes=[SR])
    P.op("dve", lambda e: e.tensor_copy(sk16[1][:], sk32[1][:]), reads=[SR], writes=[SR])

    def mk_srow(e):
        for i in range(2):
            ins = e.tensor_copy(srow[i][:].rearrange("p (h q) -> p h q", h=16), bc(sk16[i][:].unsqueeze(2), [1, 16, 128]))
        return ins
    P.op("dve", mk_srow, reads=[SR], writes=[SR])
    PTB = [[Buf("PT%d_%d" % (i, c)) for c in range(3)] for i in range(2)]
    D2 = Buf("d2")
    def att_front(it):
        j, h = it // 4, it % 4
        sset = it % 2
        pt = PT[sset]
        for c in range(3):
            bi = sset * 3 + c
            P.op("pe", lambda e, bi=bi, h=h, j=j, c=c: e.matmul(bank(bi), kT[0:64, h, (j + c) * 128:(j + c + 1) * 128],
                                                               qT[0:64, 4 * h:4 * h + 4, j * 128:(j + 1) * 128], start=True, stop=True),
                 reads=[KT[j + c], QT[j][h]], writes=[BK[bi]])
            P.op("act", lambda e, bi=bi, pt=pt, c=c: e.activation(pt[:, c, :], bank(bi), AF.Exp, scale=0.125),
                 reads=[BK[bi]], writes=[PTB[sset][c]])
        mprev = 0 if j == 0 else 1
        mnext = 3 if j == NB - 1 else 2
        for c, mi in ((0, mprev), (2, mnext)):
            P.op("pool" if c == 0 else "dve", lambda e, pt=pt, c=c, mi=mi: e.tensor_tensor(pt[:, c, :].rearrange("p (g q) -> p g q", g=4),
                                                                      pt[:, c, :].rearrange("p (g q) -> p g q", g=4),
                                                                      bc(mask_t[:, mi, :].unsqueeze(1), [128, 4, 128]), ALU.mult),
                 reads=[PTB[sset][c], CSTb], writes=[PTB[sset][c]])

    def att_back(it):
        j, h = it // 4, it % 4
        sset = it % 2
        pt = PT[sset]

        def pv(e, pt=pt, h=h, j=j):
            for c in range(3):
                e.matmul(bank(6)[0:64, :], vt[:, j + c, h * 64:(h + 1) * 64], pt[:, c, :], start=(c == 0), stop=(c == 2))
            for c in range(3):
                e.matmul(bank(7)[0:64, :], ones_bf[:, 0:64], pt[:, c, :], start=(c == 0), stop=False)
            for i in range(2):
                ins = e.matmul(bank(7)[0:64, :], ones_bf[0:1, 0:64], srow[i][0:1, 4 * h * 128:(4 * h + 4) * 128], start=False, stop=(i == 1))
            return ins
        P.op("pe", pv, reads=PTB[sset] + [VT[j], VT[j + 1], VT[j + 2], SR], writes=[BK[6], BK[7]])
        P.op("dve", lambda e: e.reciprocal(rd[0:64, :], bank(7)[0:64, :]), reads=[BK[7]], writes=[D2])
        P.op("dve", lambda e, h=h, j=j: e.tensor_tensor(qT[0:64, 4 * h:4 * h + 4, j * 128:(j + 1) * 128],
                                                        bank(6)[0:64, :].rearrange("p (g q) -> p g q", g=4),
                                                        rd[0:64, :].rearrange("p (g q) -> p g q", g=4), ALU.mult),
             reads=[BK[6], D2], writes=[QT[j][h]])

    NIT = NB * 4
    att_front(0)
    for it in range(1, NIT):
        att_front(it)
        att_back(it - 1)
    att_back(NIT - 1)
    ATT = [b for row in QT for b in row]
    if DEBUG:
        P.op("sp", lambda e: e.dma_start(out=dbg["att"], in_=qT[0:64, :, :]), reads=ATT, dma_key="dbg0")
    if STAGE <= 3:
        return finish(nc, P, out_d)

    prefetch_fence()
    a4 = Arena(nc, R_M, R_M + 20480, "a4")
    T4S = []
    for ts in range(4):
        sgl_ = a4.take("sgl%d" % ts, [128, 512], F32)
        T4S.append(dict(sgl=sgl_, xc=a4.take("xc%d" % ts, [128, 512], F32),
                        sgb=sgl_[:].bitcast(BF16), st4=a4.take("st4%d" % ts, [128, 16], F32), buf=Buf("t4_%d" % ts)))
    UT = [[Buf("uT%d_%d" % (g, th)) for th in range(2)] for g in range(8)]
    for ui in range(2):
        wt, WB = wload()
        for m in range(4):
            for th in range(2):
                bq = next_bank()

                def mmu(e, wt=wt, m=m, th=th, bq=bq):
                    for k in range(16):
                        ins = e.matmul(bank(bq), wt[:, k, m * 128:(m + 1) * 128], xnT[:, k, th * 512:(th + 1) * 512], start=(k == 0), stop=(k == 15))
                    return ins
                P.op("pe", mmu, reads=[WB] + XNT[1 + 4 * th:5 + 4 * th], writes=[BK[bq]])
                P.op("act", lambda e, ui=ui, m=m, th=th, bq=bq: e.activation(uT[:, ui * 4 + m, th * 512:(th + 1) * 512], bank(bq), AF.Gelu_apprx_tanh),
                     reads=[BK[bq]], writes=[UT[ui * 4 + m][th]])
    g4 = lambda t: t[:, 0:512].rearrange("p (g c) -> p g c", g=4)
    chains = []
    n_ch = 0
    for si in range(2):
        wl_pending = True
        for j in range(NB):
            bq = next_bank()
            C = Chain()
            if wl_pending:
                wt, WB = wload(chain=C)
                wl_pending = False
            S_ = T4S[n_ch % 4]
            n_ch += 1
            sgl, xc, sgb, st4, T4 = S_["sgl"], S_["xc"], S_["sgb"], S_["st4"], S_["buf"]

            def mms(e, wt=wt, j=j, bq=bq):
                for k in range(16):
                    ins = e.matmul(bank(bq), xnT[:, k, j * 128:(j + 1) * 128], wt[:, k, :], start=(k == 0), stop=(k == 15))
                return ins
            C.op("pe", mms, reads=[WB, XNT[j + 1]], writes=[BK[bq]])
            C.op("act", lambda e, bq=bq, sgl=sgl: e.activation(sgl[:], bank(bq), AF.Gelu_apprx_tanh), reads=[BK[bq]], writes=[T4])
            C.op("dve", lambda e, sgl=sgl, st4=st4: e.tensor_reduce(st4[:, 0:4], g4(sgl), AX.X, ALU.add), reads=[T4], writes=[T4])
            C.op("dve", lambda e, st4=st4: e.tensor_scalar(st4[:, 0:4], st4[:, 0:4], -1.0 / 128, None, ALU.mult), reads=[T4], writes=[T4])
            C.op("dve", lambda e, sgl=sgl, xc=xc, st4=st4: e.tensor_tensor(g4(xc), g4(sgl), bc(st4[:, 0:4].unsqueeze(2), [128, 4, 128]), ALU.add), reads=[T4], writes=[T4])
            C.op("act", lambda e, sgl=sgl, xc=xc: e.activation(sgl[:], xc[:], AF.Square), reads=[T4], writes=[T4])
            C.op("dve", lambda e, sgl=sgl, st4=st4: e.tensor_reduce(st4[:, 4:8], g4(sgl), AX.X, ALU.add), reads=[T4], writes=[T4])
            C.op("dve", lambda e, st4=st4: e.tensor_scalar(st4[:, 4:8], st4[:, 4:8], 1.0 / 128, EPS, ALU.mult, ALU.add), reads=[T4], writes=[T4])
            C.op("act", lambda e, st4=st4: e.activation(st4[:, 4:8], st4[:, 4:8], AF.Sqrt), reads=[T4], writes=[T4])
            C.op("dve", lambda e, st4=st4: e.reciprocal(st4[:, 8:12], st4[:, 4:8]), reads=[T4], writes=[T4])
            C.op("dve", lambda e, xc=xc, st4=st4: e.tensor_tensor(g4(xc), g4(xc), bc(st4[:, 8:12].unsqueeze(2), [128, 4, 128]), ALU.mult), reads=[T4], writes=[T4])
            C.op("pool", lambda e, si=si, xc=xc: e.tensor_tensor(xc[:], xc[:], lng_bc[:, si * 512:(si + 1) * 512], ALU.mult), reads=[T4, CSTa], writes=[T4])
            C.op("pool", lambda e, si=si, xc=xc, sgb=sgb: e.tensor_tensor(sgb[:, 0:512], xc[:], lnb_bc[:, si * 512:(si + 1) * 512], ALU.add), reads=[T4, CSTa], writes=[T4])
            bm = next_bank()

            def mix(e, si=si, bm=bm, sgb=sgb):
                for gg in range(4):
                    g = si * 4 + gg
                    e.matmul(bank(bm)[:, gg * 128:(gg + 1) * 128], sgb[:, gg * 128:(gg + 1) * 128], wsT[:, g, :], start=True, stop=False)
                    ins = e.matmul(bank(bm)[:, gg * 128:(gg + 1) * 128], ones_bf[0:1, :], bs_row[0:1, g * 128:(g + 1) * 128], start=False, stop=True)
                return ins
            C.op("pe", mix, reads=[T4, SET, CSTb], writes=[BK[bm]])
            th = j // 4
            C.op("dve", lambda e, si=si, j=j, bm=bm: e.tensor_tensor(uT[:, si * 4:si * 4 + 4, j * 128:(j + 1) * 128],
                                                                     bank(bm).rearrange("p (g t) -> p g t", g=4),
                                                                     uT[:, si * 4:si * 4 + 4, j * 128:(j + 1) * 128], ALU.mult),
                 reads=[BK[bm]], writes=[UT[si * 4 + gg][th] for gg in range(4)])
            chains.append(C)
    run_chains(chains, width=4)
    SGO = [b for row in UT for b in row]
    if DEBUG:
        P.op("sp", lambda e: e.dma_start(out=dbg["sgo"], in_=uT[:]), reads=SGO, dma_key="dbg0")
    if STAGE <= 4:
        return finish(nc, P, out_d)

    prefetch_fence()
    SA = [[Buf("sa%d_%d" % (m, th)) for th in range(2)] for m in range(4)]
    SBb = [[Buf("sb%d_%d" % (m, th)) for th in range(2)] for m in range(4)]
    MT = [[Buf("mT%d_%d" % (c, th)) for th in range(2)] for c in range(16)]
    XN_H = [XNT[1:5], XNT[5:9]]
    ATT_H = [[QT[j][h] for j in range(4 * th, 4 * th + 4) for h in range(4)] for th in range(2)]
    SGO_H = [[UT[g][th] for g in range(8)] for th in range(2)]
    for jc in range(4):
        for which, tgt, TB in ((0, sa_t, SA), (1, sb_t, SBb)):
            wt, WB = wload()
            for m in range(4):
                for th in range(2):
                    bq = next_bank()

                    def mmg(e, wt=wt, m=m, th=th, bq=bq):
                        for k in range(16):
                            ins = e.matmul(bank(bq), wt[:, k, m * 128:(m + 1) * 128], xnT[:, k, th * 512:(th + 1) * 512], start=(k == 0), stop=(k == 15))
                        return ins
                    P.op("pe", mmg, reads=[WB] + XN_H[th], writes=[BK[bq]])
                    P.op("act", lambda e, tgt=tgt, m=m, th=th, bq=bq: e.activation(tgt[:, m, th * 512:(th + 1) * 512], bank(bq), AF.Sigmoid),
                         reads=[BK[bq]], writes=[TB[m][th]])
        wt, WB = wload()
        for m in range(4):
            for th in range(2):
                bq = next_bank()

                def mma(e, wt=wt, m=m, th=th, bq=bq):
                    for hh in range(16):
                        ins = e.matmul(bank(bq), wt[0:64, hh, m * 128:(m + 1) * 128], qT[0:64, hh, th * 512:(th + 1) * 512], start=(hh == 0), stop=(hh == 15))
                    return ins
                P.op("pe", mma, reads=[WB] + ATT_H[th], writes=[BK[bq]])
                P.op("dve", lambda e, m=m, th=th, bq=bq: e.tensor_tensor(sa_t[:, m, th * 512:(th + 1) * 512], bank(bq), sa_t[:, m, th * 512:(th + 1) * 512], ALU.mult),
                     reads=[BK[bq]], writes=[SA[m][th]])
        wt, WB = wload()
        for m in range(4):
            for th in range(2):
                bq = next_bank()

                def mmb(e, wt=wt, m=m, th=th, bq=bq):
                    for g in range(8):
                        ins = e.matmul(bank(bq), wt[:, g, m * 128:(m + 1) * 128], uT[:, g, th * 512:(th + 1) * 512], start=(g == 0), stop=(g == 7))
                    return ins
                P.op("pe", mmb, reads=[WB] + SGO_H[th], writes=[BK[bq]])
                P.op("dve", lambda e, m=m, th=th, bq=bq: e.tensor_tensor(sb_t[:, m, th * 512:(th + 1) * 512], bank(bq), sb_t[:, m, th * 512:(th + 1) * 512], ALU.mult),
                     reads=[BK[bq]], writes=[SBb[m][th]])
                P.op("pool", lambda e, m=m, th=th, jc=jc: e.tensor_tensor(mT[:, jc * 4 + m, th * 512:(th + 1) * 512],
                                                                         sa_t[:, m, th * 512:(th + 1) * 512], sb_t[:, m, th * 512:(th + 1) * 512], ALU.add),
                     reads=[SA[m][th], SBb[m][th]], writes=[MT[jc * 4 + m][th]])
    MRG = [b for row in MT for b in row]
    if DEBUG:
        P.op("sp", lambda e: e.dma_start(out=dbg["mrg"], in_=mT[:]), reads=MRG, dma_key="dbg0")
    if STAGE <= 5:
        return finish(nc, P, out_d)

    prefetch_fence()
    wo, WO = [], []
    for c in range(3):
        t_, b_ = wload()
        wo.append(t_)
        WO.append(b_)
    wo.append(nc.alloc_sbuf_tensor_at("wo3", [128, 16, 512], BF16, offset=R_U))
    WO.append(Buf("wo3"))
    P.op("pool", lambda e: e.dma_start(out=wo[3][:], in_=w_out[0].rearrange("(k p) c -> p k c", p=128)[:, :, 1536:2048]),
         writes=[WO[3]], dma_key="wo3")
    a6 = Arena(nc, R_X, R_U, "a6")
    xs6 = [a6.take("xs%d" % i, [128, D], F32) for i in range(2)]
    gbc6 = a6.take("gbc", [128, D], F32)
    hn32 = [a6.take("hn32_%d" % i, [128, D], F32) for i in range(2)]
    hnb = [a6.take("hnb%d" % i, [128, D], BF16) for i in range(2)]
    hnT = [a6.take("hnT%d" % i, [128, 16, 128], F32) for i in range(2)]
    junk6 = a6.take("junk", [128, D], BF16)
    lgall = a6.take("lgall", [128, NB, 72], F32)
    a6b = Arena(nc, R_X, R_X + 24576, "a6b")
    gmx = a6b.take("gmx", [128, NB], F32)
    tsub = a6b.take("tsub", [128, NB, 8], F32)
    ohg = a6b.take("ohg", [128, NB, 8], F32)
    exg = a6b.take("exg", [128, NB, 8], F32)
    sume = a6b.take("sume", [128, NB], F32)
    gw = a6b.take("gw", [128, NB], F32)
    tmp4 = a6b.take("tmp4", [128, NB, 8, 8], F32)
    sel = a6b.take("sel", [128, NB, 8], F32)
    sel2 = a6b.take("sel2", [128, NB, 8], F32)
    v01 = a6b.take("v01", [128, 2, NB], F32)
    oh0 = a6b.take("oh0", [128, NB, 8], F32)
    oh1 = a6b.take("oh1", [128, NB, 8], F32)
    dv = a6b.take("dv", [128, NB], F32)
    s01 = a6b.take("s01", [128, 2, NB], F32)
    wk = a6b.take("wk", [128, NB, 2], F32)
    O0 = a6b.take("O0", [128, NB, 64], F32)
    O1 = a6b.take("O1", [128, NB, 64], F32)
    m0 = a6b.take("m0", [128, NB, 64], F32)
    m1 = a6b.take("m1", [128, NB, 64], F32)
    rk = a6b.take("rk", [128, NB, 2], F32)
    eidx = a6b.take("eidx", [128, NB, 2], F32)
    ovf = a6b.take("ovf", [128, NB, 2], F32)
    slotf = a6b.take("slotf", [128, NB, 2], F32)
    slotg = a6b.take("slotg", [128, NB, 2], F32)
    Ob = a6.take("Ob", [128, NB, 64], BF16)
    sloti = a6.take("sloti", [128, NB, 2], I32)
    recs = a6.take("recs", [128, NB, 2, 16], I32)
    XS6 = [Buf("xs6_0"), Buf("xs6_1")]
    HNB = [Buf("hnb0"), Buf("hnb1")]
    JK6, G6, R6, OB, REC = Buf("junk6"), Buf("gbc6"), Buf("r6"), Buf("Ob"), Buf("recs")
    H32 = [Buf("hn32_0"), Buf("hn32_1")]
    HNT = [Buf("hnT0"), Buf("hnT1")]
    OUTR = [Buf("out%d" % j) for j in range(NB)]
    P.op("sp", lambda e: e.dma_start(out=gbc6[:], in_=norm2_g.rearrange("o c -> (o c)").partition_broadcast(128)), writes=[G6], dma_key="gbc")
    P.op("pool", lambda e: e.memset(recs[:], 0), writes=[REC])
    P.op("pool", lambda e: e.tensor_copy(recs[:, :, :, 0], bc(icst_t[:, :, 0:1], [128, NB, 2])), reads=[REC, CSTa], writes=[REC])
    P.op("pool", lambda e: e.tensor_copy(recs[:, :, :, 2], icst_t[:, :, 1:3]), reads=[REC, CSTa], writes=[REC])
    chains6 = []
    for j in range(NB):
        s = j % 2
        C = Chain()
        chains6.append(C)
        half = (j % 2) * 4
        oh = 4 - half
        C.op("sp", lambda e, j=j, s=s: e.dma_start(out=xs6[s][:], in_=xh[(j + 1) * 128:(j + 2) * 128, :]), writes=[XS6[s]], dma_key="xs6_%d" % s)

        for c in range(4):
            def mmo(e, j=j, half=half, c=c):
                for k in range(16):
                    ins = e.matmul(bank(half + c), mT[:, k, j * 128:(j + 1) * 128], wo[c][:, k, :], start=(k == 0), stop=(k == 15))
                return ins
            C.op("pe", mmo, reads=[WO[c]] + [MT[k][j // 4] for k in range(16)], writes=[BK[half + c]])
        for c in range(4):
            C.op("dve", lambda e, s=s, half=half, c=c: e.tensor_tensor(xs6[s][:, c * 512:(c + 1) * 512], bank(half + c), xs6[s][:, c * 512:(c + 1) * 512], ALU.add),
                 reads=[BK[half + c], XS6[s]], writes=[XS6[s]])
        C.op("sp", lambda e, j=j, s=s: e.dma_start(out=out_d[j * 128:(j + 1) * 128, :], in_=xs6[s][:]), reads=[XS6[s]], writes=[OUTR[j]], dma_key="h_st%d" % s)
        C.op("act", lambda e, j=j, s=s: e.activation(junk6[:], xs6[s][:], AF.Square, accum_out=ss_t[:, 16 + j:17 + j]),
             reads=[XS6[s]], writes=[JK6, SSC[16 + j]])
        rstd_chain(C, 16 + j, 1.0 / D)
        C.op("dve", lambda e, j=j, s=s: e.scalar_tensor_tensor(hn32[s][:], xs6[s][:], rstd_t[:, 16 + j:17 + j], gbc6[:], ALU.mult, ALU.mult),
             reads=[XS6[s], SSC[16 + j], G6], writes=[H32[s]])
        C.op("act", lambda e, s=s: e.activation(hnb[s][:], hn32[s][:], AF.Copy), reads=[H32[s]], writes=[HNB[s]])
        C.op("sp", lambda e, j=j, s=s: e.dma_start(out=hn_d[j * 128:(j + 1) * 128, :], in_=hnb[s][:]), reads=[HNB[s]], writes=[HND], dma_key="hn_st%d" % s)
        for q4 in range(4):
            bi = oh + q4

            def tr6(e, q4=q4, bi=bi, s=s):
                for kq in range(4):
                    k = q4 * 4 + kq
                    ins = e.transpose(bank(bi)[:, kq * 128:(kq + 1) * 128], hn32[s][:, k * 128:(k + 1) * 128], ident_f)
                return ins
            C.op("pe", tr6, reads=[H32[s], CSTa], writes=[BK[bi]])
            src = bank(bi).rearrange("p (k t) -> p k t", k=4)
            if q4 % 2 == 0:
                C.op("act", lambda e, q4=q4, src=src, s=s: e.activation(hnT[s][:, q4 * 4:q4 * 4 + 4, :], src, AF.Copy), reads=[BK[bi]], writes=[HNT[s]])
            else:
                C.op("dve", lambda e, q4=q4, src=src, s=s: e.tensor_copy(hnT[s][:, q4 * 4:q4 * 4 + 4, :], src), reads=[BK[bi]], writes=[HNT[s]])
        br_ = oh

        def mmr(e, br_=br_, s=s):
            for k in range(16):
                ins = e.matmul(bank(br_)[:, 0:72], hnT[s][:, k, :], Wr[:, k, :], start=(k == 0), stop=(k == 15))
            return ins
        C.op("pe", mmr, reads=[HNT[s], CSTa], writes=[BK[br_]])
        C.op("dve", lambda e, br_=br_, j=j: e.tensor_tensor(lgall[:, j, :], bank(br_)[:, 0:72], br_bc[:], ALU.add), reads=[BK[br_], CSTa], writes=[R6])

    run_chains(chains6, width=2)
    P.fence()

    EW = []
    for i, base in enumerate((R_RING, R_U, MAIN0)):
        ea = Arena(nc, base, base + 49152, "ew%d" % i)
        EW.append((ea.take("eg", [128, 16, FF], BF16), ea.take("eu", [128, 16, FF], BF16), ea.take("ed", [128, 4, D], BF16)))
    EWB = [(Buf("eg%d" % i), Buf("eu%d" % i), Buf("ed%d" % i)) for i in range(3)]

    def eload(ex):
        i = ex % 3
        g_t, u_t, d_t = EW[i]
        P.op("pool", lambda e: e.dma_start(out=g_t[:], in_=w_gate[0, ex].rearrange("(p k) f -> p k f", p=128), max_dma_last_dim=8192),
             writes=[EWB[i][0]], dma_key="eg%d" % i)
        P.op("pool", lambda e: e.dma_start(out=u_t[:], in_=w_up[0, ex].rearrange("(p k) f -> p k f", p=128), max_dma_last_dim=8192),
             writes=[EWB[i][1]], dma_key="eu%d" % i)
        P.op("pool", lambda e: e.dma_start(out=d_t[:], in_=w_down[0, ex].rearrange("(p k) c -> p k c", p=128), max_dma_last_dim=8192),
             writes=[EWB[i][2]], dma_key="ed%d" % i)

    if STAGE >= 7:
        eload(0)
        eload(1)

    gl3 = lgall[:, :, 0:8]
    el4 = lgall[:, :, 8:72].rearrange("p j (g e) -> p j g e", g=8)
    B3 = [128, NB, 8]
    B4 = [128, NB, 8, 8]
    rop = lambda eng, fn: P.op(eng, fn, reads=[R6], writes=[R6])
    rop("dve", lambda e: e.tensor_reduce(gmx[:], gl3, AX.X, ALU.max))
    rop("dve", lambda e: e.tensor_tensor(tsub[:], gl3, bc(gmx[:].unsqueeze(2), B3), ALU.subtract))
    rop("dve", lambda e: e.tensor_scalar(ohg[:], tsub[:], 0.0, None, ALU.is_equal))
    rop("act", lambda e: e.activation(exg[:], tsub[:], AF.Exp))
    rop("dve", lambda e: e.tensor_reduce(sume[:], exg[:], AX.X, ALU.add))
    rop("dve", lambda e: e.reciprocal(gw[:], sume[:]))
    rop("dve", lambda e: e.tensor_tensor(tmp4[:], el4, bc(ohg[:].unsqueeze(3), B4), ALU.mult))
    rop("dve", lambda e: e.tensor_reduce(sel[:], tmp4[:].rearrange("p j g e -> p j e g"), AX.X, ALU.add))
    rop("dve", lambda e: e.tensor_reduce(v01[:, 0, :], sel[:], AX.X, ALU.max))
    rop("dve", lambda e: e.tensor_tensor(oh0[:], sel[:], bc(v01[:, 0, :].unsqueeze(2), B3), ALU.is_equal))
    rop("dve", lambda e: e.scalar_tensor_tensor(sel2[:], oh0[:], -1.0e30, sel[:], ALU.mult, ALU.add))
    rop("dve", lambda e: e.tensor_reduce(v01[:, 1, :], sel2[:], AX.X, ALU.max))

    def r_a(e):
        e.tensor_tensor(oh1[:], sel2[:], bc(v01[:, 1, :].unsqueeze(2), B3), ALU.is_equal)
        return e.tensor_tensor(dv[:], v01[:, 0, :], v01[:, 1, :], ALU.subtract)
    rop("dve", r_a)

    def r_b(e):
        e.activation(s01[:, 0, :], dv[:], AF.Sigmoid)
        return e.activation(s01[:, 1, :], dv[:], AF.Sigmoid, scale=-1.0)
    rop("act", r_b)

    def r_c(e):
        e.tensor_tensor(wk[:, :, 0], s01[:, 0, :], gw[:], ALU.mult)
        e.tensor_tensor(wk[:, :, 1], s01[:, 1, :], gw[:], ALU.mult)
        e.tensor_tensor(O0[:].rearrange("p j (g e) -> p j g e", g=8), bc(ohg[:].unsqueeze(3), B4), bc(oh0[:].unsqueeze(2), B4), ALU.mult)
        return e.tensor_tensor(O1[:].rearrange("p j (g e) -> p j g e", g=8), bc(ohg[:].unsqueeze(3), B4), bc(oh1[:].unsqueeze(2), B4), ALU.mult)
    rop("dve", r_c)
    P.op("dve", lambda e: e.tensor_tensor(Ob[:], O0[:], O1[:], ALU.add), reads=[R6], writes=[OB])
    bp = 0

    def pre(e):
        for j in range(NB):
            o_ = bank(bp)[:, j * 64:(j + 1) * 64]
            ins = e.matmul(o_, U_bf[:], Ob[:, j, :], start=True, stop=(j == 0))
            for jj in range(j):
                ins = e.matmul(o_, ones_bf[:], Ob[:, jj, :], start=False, stop=(jj == j - 1))
        return ins
    P.op("pe", pre, reads=[OB, SET], writes=[BK[bp]])
    pre3 = bank(bp).rearrange("p (j e) -> p j e", j=NB)

    def r_d(e):
        e.tensor_tensor(m0[:], O0[:], pre3, ALU.mult)
        return e.tensor_tensor(m1[:], O1[:], pre3, ALU.mult)
    P.op("dve", r_d, reads=[R6, BK[bp]], writes=[R6])

    def r_e(e):
        e.tensor_reduce(rk[:, :, 0], m0[:], AX.X, ALU.add)
        return e.tensor_reduce(rk[:, :, 1], m1[:], AX.X, ALU.add)
    rop("dve", r_e)

    def r_f(e):
        e.tensor_tensor(m0[:], O0[:], bc(iota64.unsqueeze(1), [128, NB, 64]), ALU.mult)
        return e.tensor_tensor(m1[:], O1[:], bc(iota64.unsqueeze(1), [128, NB, 64]), ALU.mult)
    P.op("dve", r_f, reads=[R6, CSTa], writes=[R6])

    def r_g(e):
        e.tensor_reduce(eidx[:, :, 0], m0[:], AX.X, ALU.add)
        e.tensor_reduce(eidx[:, :, 1], m1[:], AX.X, ALU.add)
        return e.tensor_scalar(ovf[:], rk[:], float(CAP) - 0.5, 1.0e9, ALU.is_gt, ALU.mult)
    rop("dve", r_g)
    rop("dve", lambda e: e.scalar_tensor_tensor(slotf[:], eidx[:], float(CAP), rk[:], ALU.mult, ALU.add))
    rop("dve", lambda e: e.tensor_tensor(slotg[:], slotf[:], ovf[:], ALU.add))

    def r_h(e):
        e.tensor_copy(sloti[:], slotg[:])
        return e.tensor_copy(recs[:, :, :, 1], wk[:].bitcast(I32))
    P.op("dve", r_h, reads=[R6, REC], writes=[R6, REC])
    def tbl_scatter(e):
        r = []
        for j in range(NB):
            for k in range(2):
                r.append(e.indirect_dma_start(out=tbl_d, out_offset=bass.IndirectOffsetOnAxis(ap=sloti[:, j, k:k + 1], axis=0),
                                              in_=recs[:, j, k, :], in_offset=None, bounds_check=bnd_tbl, oob_is_err=False))
        return r
    P.op("pool", tbl_scatter, reads=[R6, REC], writes=[TBL], dma_key="tblsc", ndma=2 * NB)
    if DEBUG:
        P.op("pool", lambda e: e.dma_start(out=dbg["tbl"], in_=tbl_d), reads=[TBL], dma_key="dbg1")
        P.op("pool", lambda e: e.dma_start(out=dbg["hn"], in_=hn_d), reads=[HND], dma_key="dbg1")
    if STAGE <= 6:
        return finish(nc, P, out_d, extra=OUTR)

    P.fence()
    a7 = Arena(nc, MAIN0 + 49152, MAIN0 + 81920, "a7")
    xg = [a7.take("xg%d" % i, [128, D], BF16) for i in range(2)]
    xeT = [a7.take("xeT%d" % i, [128, 16, 128], BF16) for i in range(2)]
    ys = [a7.take("ys%d" % i, [128, D], F32) for i in range(2)]
    a7t = Arena(nc, R_RING + 49152, SB_END, "a7t")
    hT = [a7t.take("hT%d" % i, [128, 4, 128], BF16) for i in range(2)]
    sgt = a7t.take("sgt", [128, 512], F32)
    XG = [Buf("xg0"), Buf("xg1")]
    XET = [Buf("xeT0"), Buf("xeT1")]
    HT = [Buf("hT0"), Buf("hT1")]
    SGT = Buf("sgt")
    YS = [Buf("ys0"), Buf("ys1")]
    TBE = [Buf("tbe%d" % i) for i in range(4)]
    Y2S = [Buf("Y2_0"), Buf("Y2_1")]
    for i in range(2):
        P.op("pool", lambda e, i=i: e.memset(xg[i][:], 0.0), writes=[XG[i]])

    def egather(ex):
        s = ex % 2
        s4 = ex % 4
        P.op("sp", lambda e: e.dma_start(out=tbe[s4][:], in_=tbl_d[ex * CAP:(ex + 1) * CAP, :]), reads=[TBL], writes=[TBE[s4]], dma_key="tbe%d" % s4)
        P.op("pool", lambda e: e.indirect_dma_start(out=xg[s][:], out_offset=None, in_=hn_d,
                                                    in_offset=bass.IndirectOffsetOnAxis(ap=tbe[s4][:, 0:1], axis=0),
                                                    bounds_check=bnd_tok, oob_is_err=False),
             reads=[TBE[s4], HND], writes=[XG[s]], dma_key="xg%d" % s)

    egather(0)
    for ex in range(NE):
        s = ex % 2
        i3 = ex % 3
        g_t, u_t, d_t = EW[i3]
        if ex + 2 < NE:
            eload(ex + 2)
        if ex + 1 < NE:
            egather(ex + 1)
        for hf in range(2):
            bi = hf

            def tre(e, s=s, hf=hf, bi=bi):
                pb = bank(bi).bitcast(BF16)
                for kq in range(8):
                    k = hf * 8 + kq
                    ins = e.transpose(pb[:, kq * 128:(kq + 1) * 128], xg[s][:, k:D:16], ident_bf[:])
                return ins
            P.op("pe", tre, reads=[XG[s], SET], writes=[BK[bi]])
            src = bank(bi).bitcast(BF16).rearrange("p (k t) -> p k t", k=8)
            if hf == 0:
                P.op("act", lambda e, s=s, hf=hf, src=src: e.activation(xeT[s][:, hf * 8:hf * 8 + 8, :], src, AF.Copy), reads=[BK[bi]], writes=[XET[s]])
            else:
                P.op("dve", lambda e, s=s, hf=hf, src=src: e.tensor_copy(xeT[s][:, hf * 8:hf * 8 + 8, :], src), reads=[BK[bi]], writes=[XET[s]])

        def gu(e, s=s, g_t=g_t, u_t=u_t):
            for (wt_, bi) in ((g_t, 2), (u_t, 3)):
                for m in range(4):
                    for k in range(16):
                        ins = e.matmul(bank(bi)[:, m * 128:(m + 1) * 128], wt_[:, k, m:FF:4], xeT[s][:, k, :], start=(k == 0), stop=(k == 15))
            return ins
        P.op("pe", gu, reads=[XET[s], EWB[i3][0], EWB[i3][1]], writes=[BK[2], BK[3]])
        P.op("act", lambda e: e.activation(sgt[:], bank(2), AF.Silu), reads=[BK[2]], writes=[SGT])
        P.op("dve", lambda e, s=s: e.tensor_tensor(hT[s][:].rearrange("p m t -> p (m t)"), sgt[:], bank(3), ALU.mult), reads=[SGT, BK[3]], writes=[HT[s]])

        def dn(e, s=s, d_t=d_t):
            for c in range(4):
                for m in range(4):
                    ins = e.matmul(bank(4 + c), hT[s][:, m, :], d_t[:, m, c * 512:(c + 1) * 512], start=(m == 0), stop=(m == 3))
            return ins
        P.op("pe", dn, reads=[HT[s], EWB[i3][2]], writes=BK[4:8])
        s4 = ex % 4
        wcol = tbe[s4][:, 1:2].bitcast(F32)
        P.op("act", lambda e, s=s, wcol=wcol: e.activation(ys[s][:, 0:1024], psB[:, 0:1024], AF.Identity, scale=wcol), reads=[BK[4], BK[5], TBE[s4]], writes=[YS[s]])
        P.op("dve", lambda e, s=s, wcol=wcol: e.tensor_scalar(ys[s][:, 1024:2048], psB[:, 1024:2048], wcol, None, ALU.mult), reads=[BK[6], BK[7], TBE[s4]], writes=[YS[s]])
        def ysc(e, s=s, s4=s4):
            return [e.indirect_dma_start(out=y2_d[hf], out_offset=bass.IndirectOffsetOnAxis(ap=tbe[s4][:, 2:3], axis=0),
                                         in_=ys[s][:, hf * 1024:(hf + 1) * 1024], in_offset=None,
                                         bounds_check=bnd_y2, oob_is_err=False) for hf in range(2)]
        P.op("pool", ysc, reads=[YS[s], TBE[s4]], writes=[Y2S[s]], dma_key="ysc%d" % s, ndma=2)

    P.fence()
    a8 = Arena(nc, MAIN0, SB_END, "a8")
    NS8 = 4
    hh = [a8.take("hh%d" % i, [128, D], F32) for i in range(NS8)]
    yy = [a8.take("yy%d" % i, [128, 2, D], F32) for i in range(NS8)]
    HH = [Buf("hh%d" % i) for i in range(NS8)]
    YY = [Buf("yy%d" % i) for i in range(NS8)]
    fin = []
    chains8 = []
    for j in range(NB):
        s = j % NS8
        C = Chain()
        chains8.append(C)
        C.op("sp", lambda e, j=j, s=s: e.dma_start(out=hh[s][:], in_=out_d[j * 128:(j + 1) * 128, :]), reads=[OUTR[j]], writes=[HH[s]], dma_key="hh%d" % s)

        def ldy(e, j=j, s=s):
            r = []
            for hf in range(2):
                r.append(e.dma_start(out=yy[s][:, :, hf * 1024:(hf + 1) * 1024],
                                     in_=y2_d[hf].rearrange("(t k) d -> t k d", k=2)[j * 128:(j + 1) * 128, :, :]))
            return r
        C.op("sp", ldy, reads=Y2S, writes=[YY[s]], dma_key="yy%d" % s, ndma=2)
        C.op("dve", lambda e, s=s: e.tensor_tensor(hh[s][:], hh[s][:], yy[s][:, 0, :], ALU.add), reads=[YY[s]], writes=[HH[s]])
        C.op("dve", lambda e, s=s: e.tensor_tensor(hh[s][:], hh[s][:], yy[s][:, 1, :], ALU.add), reads=[YY[s]], writes=[HH[s]])
        C.op("act", lambda e, j=j, s=s: e.dma_start(out=out_d[j * 128:(j + 1) * 128, :], in_=hh[s][:]), reads=[HH[s]], writes=[OUTR[j]], dma_key="o_st%d" % s)
    run_chains(chains8, width=3)
    fw = [("act", P.dma_keys["o_st%d" % i][-1]) for i in range(NS8)]
    for key in ("dbg0", "dbg1"):
        if key in P.dma_keys:
            fw.append((P.dma_keys[key][-1].eng, P.dma_keys[key][-1]))
    P.emit(final_waits=fw)
    return nc


def finish(nc, P, out_d, extra=None):
    fw = []
    for key, lst in P.dma_keys.items():
        fw.append((lst[-1].eng, lst[-1]))
    P.emit(final_waits=fw)
    return nc


_CACHE = {}


def _consts():
    cst = np.zeros((128, 352), np.float32)
    inv = (500000.0 ** (-np.arange(0, 16, 2, dtype=np.float32) / 16.0)).astype(np.float32)
    cst[:, 0:8] = inv
    cst[:, 8:16] = inv
    cst[:, 24:32] = np.float32(math.pi / 2)
    cst[:, 32:96] = np.arange(64, dtype=np.float32)
    cst[:, 96:224] = np.eye(128, dtype=np.float32)
    cst[:, 224:352] = np.triu(np.ones((128, 128), np.float32), 1)
    icst = np.zeros((128, NB, 3), np.int32)
    tok = np.arange(NB)[None, :] * 128 + np.arange(128)[:, None]
    icst[:, :, 0] = tok
    icst[:, :, 1] = tok * 2
    icst[:, :, 2] = tok * 2 + 1
    return cst, icst


def kernel(**inputs):
    x = np.asarray(inputs["x"], np.float32)
    pos = np.asarray(inputs["positions"], np.int32)
    B, S, _ = x.shape
    if "nc" not in _CACHE:
        _CACHE["nc"] = build_program()
    nc = _CACHE["nc"]
    cst, icst = _consts()
    kk = np.arange(128)[:, None]
    ii = np.arange(128)[None, :]
    mprev = (kk >= ii).astype(np.float32)
    mnext = (kk <= ii).astype(np.float32)
    shared = {}
    for k in ("norm1_g", "w_in", "q_norm_g", "k_norm_g", "sink_logits", "sg_ln_g", "sg_ln_b", "sg_w", "sg_b", "w_branch_att",
              "w_branch_sg", "w_out", "norm2_g", "w_group_router", "b_group_router", "w_expert_router", "b_expert_router",
              "w_gate", "w_up", "w_down"):
        if STAGE < 7 and k in ("w_gate", "w_up", "w_down"):
            continue
        shared[k] = np.ascontiguousarray(np.asarray(inputs[k], np.float32))
    in_maps = []
    for c in range(NCORES):
        b, half = c // 2, c % 2
        t0 = half * TOK
        xh = np.zeros((NBH * 128, D), np.float32)
        ph = np.zeros((NBH * 128,), np.int32)
        lo, hi = t0 - 128, t0 + TOK + 128
        slo, shi = max(lo, 0), min(hi, S)
        xh[slo - lo:shi - lo] = x[b, slo:shi]
        ph[slo - lo:shi - lo] = pos[b, slo:shi]
        masks = np.zeros((128, 4, 128), np.float32)
        masks[:, 0, :] = mprev * (1.0 if lo >= 0 else 0.0)
        masks[:, 1, :] = mprev
        masks[:, 2, :] = mnext
        masks[:, 3, :] = mnext * (1.0 if hi <= S else 0.0)
        m = dict(shared)
        m.update({"xh": xh, "posT": np.ascontiguousarray(ph.reshape(NBH, 128).T), "masks": masks, "cst": cst, "icst": icst})
        in_maps.append(m)
    res = run_bass_kernel_spmd(nc, in_maps, core_ids=list(range(NCORES)))
    _CACHE["res"] = res
    out = np.zeros((B, S, D), np.float32)
    for c in range(NCORES):
        b, half = c // 2, c % 2
        out[b, half * TOK:(half + 1) * TOK] = np.asarray(res.results[c]["out"], np.float32)
    return out
```
